# Optimizing a Trainium2 kernel written in Bass

```python
import math
import jax
import jax.numpy as jnp
from jax import lax
import numpy as np


D_MODEL = 1024
BATCH = 8
SEQ = 4096
DEPTH = 4

HEAD_DIM = 64
N_Q_HEADS = 8
N_KV_HEADS = 2
Q_PER_KV = N_Q_HEADS // N_KV_HEADS
ATTN_WIDTH = N_Q_HEADS * HEAD_DIM
KV_WIDTH = N_KV_HEADS * HEAD_DIM
N_BRANCH = 3
SSM_WIDTH = D_MODEL - ATTN_WIDTH
SSM_GROUP = 16
N_SSM_GROUPS = SSM_WIDTH // SSM_GROUP
SSM_STATE = 64
MIX_WIDTH = ATTN_WIDTH + SSM_WIDTH
PROJ_WIDTH = ATTN_WIDTH + 6 * KV_WIDTH + N_BRANCH * N_Q_HEADS + SSM_WIDTH

CMP_LEN = 32
CMP_STRIDE = 16
SLC_LEN = 64
N_SEL = 16
WINDOW = 512
Q_BLOCK = 128

DT_MIN = 1e-3
DT_MAX = 1e-1

D_FF = 2816
N_EXPERTS = 8
TOP_K = 2
N_DENSE = (DEPTH + 1) // 2
N_MOE = DEPTH // 2

DEEPNORM_ALPHA = (2.0 * DEPTH) ** 0.25
DEEPNORM_BETA = (8.0 * DEPTH) ** -0.25
LN_EPS = 1e-5
RMS_EPS = 1e-6

kernel_name = 'hybrid_nsa_s5_moe_deepnorm_block'


def layer_norm(x, g, b):
    x32 = x.astype(jnp.float32)
    mu = jnp.mean(x32, axis=-1, keepdims=True)
    var = jnp.mean(jnp.square(x32 - mu), axis=-1, keepdims=True)
    y = (x32 - mu) * lax.rsqrt(var + LN_EPS)
    return (y * g.astype(jnp.float32) + b.astype(jnp.float32)).astype(x.dtype)


def rms_norm(x, g):
    x32 = x.astype(jnp.float32)
    y = x32 * lax.rsqrt(jnp.mean(jnp.square(x32), axis=-1, keepdims=True) + RMS_EPS)
    return (y * g.astype(jnp.float32)).astype(x.dtype)


def masked_softmax(s, mask):
    s = jnp.where(mask, s.astype(jnp.float32), -jnp.inf)
    m = jnp.max(s, axis=-1, keepdims=True)
    m = jnp.where(jnp.isfinite(m), m, 0.0)
    e = jnp.exp(s - m)
    den = jnp.sum(e, axis=-1, keepdims=True)
    return e / jnp.where(den > 0, den, 1.0)


def swiglu(h, w_gate, w_up, w_down):
    return (jax.nn.silu(h @ w_gate) * (h @ w_up)) @ w_down


def moe_swiglu(h, router, w_gate, w_up, w_down):
    logits = (h @ router).astype(jnp.float32)
    top_val, top_idx = lax.top_k(logits, TOP_K)
    top_w = jax.nn.softmax(top_val, axis=-1)
    gate = jnp.sum(jax.nn.one_hot(top_idx, N_EXPERTS, dtype=jnp.float32) * top_w[..., None],
                   axis=-2).astype(h.dtype)
    out = jnp.zeros_like(h)
    for e in range(N_EXPERTS):
        out = out + gate[..., e:e + 1] * swiglu(h, w_gate[e], w_up[e], w_down[e])
    return out


def compress_blocks(k, pos, w1, w2):
    L = k.shape[1]
    n_cmp = (L - CMP_LEN) // CMP_STRIDE + 1
    idx = np.arange(n_cmp)[:, None] * CMP_STRIDE + np.arange(CMP_LEN)[None, :]
    blocks = k[:, idx] + pos[None, None, :, None, :]
    hid = jax.nn.gelu(jnp.einsum('bnlhd,ldf->bnhf', blocks, w1))
    return jnp.einsum('bnhf,fd->bnhd', hid, w2)


def nsa_attention(q, k_cmp, v_cmp, k_slc, v_slc, k_win, v_win, gates):
    B, L = q.shape[0], q.shape[1]
    scale = HEAD_DIM ** -0.5
    n_cmp = k_cmp.shape[1]
    n_slc = L // SLC_LEN
    n_sel = min(N_SEL, n_slc)
    cmp_end = jnp.arange(n_cmp) * CMP_STRIDE + CMP_LEN - 1
    cs = np.arange(n_cmp)[:, None] * CMP_STRIDE
    ss = np.arange(n_slc)[None, :] * SLC_LEN
    overlap = jnp.asarray((cs < ss + SLC_LEN) & (cs + CMP_LEN > ss), jnp.float32)
    kb = k_slc.reshape(B, n_slc, SLC_LEN, N_KV_HEADS, HEAD_DIM).transpose(0, 3, 1, 2, 4)
    vb = v_slc.reshape(B, n_slc, SLC_LEN, N_KV_HEADS, HEAD_DIM).transpose(0, 3, 1, 2, 4)
    kw = jnp.pad(k_win, ((0, 0), (WINDOW, 0), (0, 0), (0, 0)))
    vw = jnp.pad(v_win, ((0, 0), (WINDOW, 0), (0, 0), (0, 0)))
    gather = jax.vmap(jax.vmap(lambda blocks, idx: blocks[idx]))
    blk = jnp.arange(n_slc)

    def block(i):
        qs = i * Q_BLOCK
        qb = lax.dynamic_slice_in_dim(q, qs, Q_BLOCK, axis=1) * scale
        gb = lax.dynamic_slice_in_dim(gates, qs, Q_BLOCK, axis=1)
        t = qs + jnp.arange(Q_BLOCK)

        s = jnp.einsum('bqhgd,bnhd->bhgqn', qb, k_cmp)
        p_cmp = masked_softmax(s, cmp_end[None, :] <= t[:, None])
        o_cmp = jnp.einsum('bhgqn,bnhd->bqhgd', p_cmp.astype(v_cmp.dtype), v_cmp)

        imp = jnp.einsum('bhgqn,ns->bhqs', p_cmp, overlap)
        cur = t // SLC_LEN
        forced = (blk[None, :] == 0) | (blk[None, :] == cur[:, None]) | (blk[None, :] == cur[:, None] - 1)
        causal = blk[None, :] * SLC_LEN <= t[:, None]
        score = jnp.where(causal, jnp.where(forced, jnp.inf, imp), -jnp.inf)
        top_val, top_idx = lax.top_k(score, n_sel)
        sel_valid = top_val > -jnp.inf
        k_sel = gather(kb, top_idx)
        v_sel = gather(vb, top_idx)
        kpos = top_idx[..., None] * SLC_LEN + jnp.arange(SLC_LEN)
        m_sel = sel_valid[..., None] & (kpos <= t[None, None, :, None, None])
        s = jnp.einsum('bqhgd,bhqjld->bhgqjl', qb, k_sel).reshape(B, N_KV_HEADS, Q_PER_KV, Q_BLOCK, n_sel * SLC_LEN)
        p = masked_softmax(s, m_sel.reshape(B, N_KV_HEADS, Q_BLOCK, n_sel * SLC_LEN)[:, :, None])
        o_slc = jnp.einsum('bhgqk,bhqkd->bqhgd', p.astype(v_sel.dtype),
                           v_sel.reshape(B, N_KV_HEADS, Q_BLOCK, n_sel * SLC_LEN, HEAD_DIM))

        kwb = lax.dynamic_slice_in_dim(kw, qs, WINDOW + Q_BLOCK, axis=1)
        vwb = lax.dynamic_slice_in_dim(vw, qs, WINDOW + Q_BLOCK, axis=1)
        wpos = qs - WINDOW + jnp.arange(WINDOW + Q_BLOCK)
        dist = t[:, None] - wpos[None, :]
        m_win = (dist >= 0) & (dist < WINDOW) & (wpos[None, :] >= 0)
        s = jnp.einsum('bqhgd,bkhd->bhgqk', qb, kwb)
        p = masked_softmax(s, m_win)
        o_win = jnp.einsum('bhgqk,bkhd->bqhgd', p.astype(vwb.dtype), vwb)

        g = jax.nn.sigmoid(gb.astype(jnp.float32))
        o = (g[..., 0:1] * o_cmp.astype(jnp.float32) + g[..., 1:2] * o_slc.astype(jnp.float32)
             + g[..., 2:3] * o_win.astype(jnp.float32))
        return o.astype(q.dtype)

    out = lax.map(block, jnp.arange(L // Q_BLOCK))
    return out.transpose(1, 0, 2, 3, 4, 5).reshape(B, L, ATTN_WIDTH)


def cplx(re, im):
    return lax.complex(re.astype(jnp.float32), im.astype(jnp.float32))


def s5_ssm(u, a_re, a_im, log_dt, b_re, b_im, c_re, c_im, d_skip):
    B, L = u.shape[0], u.shape[1]
    u32 = u.astype(jnp.float32).reshape(B, L, N_SSM_GROUPS, SSM_GROUP)
    a = cplx(a_re, a_im)
    dt = jnp.exp(log_dt.astype(jnp.float32))[:, None]
    a_bar = jnp.exp(a * dt)
    b_bar = ((a_bar - 1.0) / a)[..., None] * cplx(b_re, b_im)
    bu = jnp.einsum('blgh,gph->blgp', u32.astype(jnp.complex64), b_bar)
    a_seq = jnp.broadcast_to(a_bar, (1, L) + a_bar.shape)

    def combine(left, right):
        return (right[0] * left[0], right[0] * left[1] + right[1])

    _, states = lax.associative_scan(combine, (a_seq, bu), axis=1)
    y = jnp.einsum('blgp,ghp->blgh', states, cplx(c_re, c_im)).real + d_skip.astype(jnp.float32) * u32
    return y.reshape(B, L, SSM_WIDTH)


def hybrid_mixer(h, w_in, cmp_pos_k, cmp_pos_v, cmp_w1_k, cmp_w2_k, cmp_w1_v, cmp_w2_v,
                 a_re, a_im, log_dt, b_re, b_im, c_re, c_im, d_skip, w_glu, norm_attn, norm_ssm, w_out):
    B, L = h.shape[0], h.shape[1]
    sizes = [ATTN_WIDTH] + [KV_WIDTH] * 6 + [N_BRANCH * N_Q_HEADS, SSM_WIDTH]
    cuts = np.cumsum(sizes)[:-1].tolist()
    q, kc, vc, ks, vs, kw, vw, g, u = jnp.split(h @ w_in, cuts, axis=-1)

    def kv(t):
        return t.reshape(B, L, N_KV_HEADS, HEAD_DIM)

    k_cmp = compress_blocks(kv(kc), cmp_pos_k, cmp_w1_k, cmp_w2_k)
    v_cmp = compress_blocks(kv(vc), cmp_pos_v, cmp_w1_v, cmp_w2_v)
    o_attn = nsa_attention(q.reshape(B, L, N_KV_HEADS, Q_PER_KV, HEAD_DIM), k_cmp, v_cmp,
                           kv(ks), kv(vs), kv(kw), kv(vw),
                           g.reshape(B, L, N_KV_HEADS, Q_PER_KV, N_BRANCH))
    y = jax.nn.gelu(s5_ssm(u, a_re, a_im, log_dt, b_re, b_im, c_re, c_im, d_skip).astype(h.dtype))
    o_ssm = y * jax.nn.sigmoid(y @ w_glu)
    o = jnp.concatenate([rms_norm(o_attn, norm_attn), rms_norm(o_ssm, norm_ssm)], axis=-1)
    return o @ w_out


def setup_inputs(seed: int = 0) -> dict:
    key = jax.random.key(seed)
    keys = jax.random.split(key, 32)

    def nrm(i, shape, scale):
        return scale * jax.random.normal(keys[i], shape, jnp.float32)

    G, P, H = N_SSM_GROUPS, SSM_STATE, SSM_GROUP
    n_idx = jnp.arange(P, dtype=jnp.float32)
    return {
        'x': nrm(0, (BATCH, SEQ, D_MODEL), 1.0),
        'c': nrm(1, (BATCH, D_MODEL), 1.0),
        'w_in': nrm(2, (DEPTH, D_MODEL, PROJ_WIDTH), D_MODEL ** -0.5),
        'cmp_pos_k': nrm(3, (DEPTH, CMP_LEN, HEAD_DIM), 0.1),
        'cmp_pos_v': nrm(4, (DEPTH, CMP_LEN, HEAD_DIM), 0.1),
        'cmp_w1_k': nrm(5, (DEPTH, CMP_LEN, HEAD_DIM, HEAD_DIM), (CMP_LEN * HEAD_DIM) ** -0.5),
        'cmp_w2_k': nrm(6, (DEPTH, HEAD_DIM, HEAD_DIM), HEAD_DIM ** -0.5),
        'cmp_w1_v': nrm(7, (DEPTH, CMP_LEN, HEAD_DIM, HEAD_DIM), (CMP_LEN * HEAD_DIM) ** -0.5),
        'cmp_w2_v': nrm(8, (DEPTH, HEAD_DIM, HEAD_DIM), HEAD_DIM ** -0.5),
        'ssm_a_re': -0.5 + nrm(9, (DEPTH, G, P), 0.01),
        'ssm_a_im': math.pi * n_idx + nrm(10, (DEPTH, G, P), 0.01),
        'ssm_log_dt': jax.random.uniform(keys[11], (DEPTH, G), jnp.float32, math.log(DT_MIN), math.log(DT_MAX)),
        'ssm_b_re': nrm(12, (DEPTH, G, P, H), (2.0 * H) ** -0.5),
        'ssm_b_im': nrm(13, (DEPTH, G, P, H), (2.0 * H) ** -0.5),
        'ssm_c_re': nrm(14, (DEPTH, G, H, P), (2.0 * P) ** -0.5),
        'ssm_c_im': nrm(15, (DEPTH, G, H, P), (2.0 * P) ** -0.5),
        'ssm_d': nrm(16, (DEPTH, G, H), 1.0),
        'ssm_w_glu': nrm(17, (DEPTH, SSM_WIDTH, SSM_WIDTH), SSM_WIDTH ** -0.5),
        'norm_attn': 1.0 + nrm(18, (DEPTH, ATTN_WIDTH), 0.01),
        'norm_ssm': 1.0 + nrm(19, (DEPTH, SSM_WIDTH), 0.01),
        'w_out': nrm(20, (DEPTH, MIX_WIDTH, D_MODEL), DEEPNORM_BETA * MIX_WIDTH ** -0.5),
        'ada_w': nrm(21, (DEPTH, D_MODEL, 6 * D_MODEL), 0.1 * D_MODEL ** -0.5),
        'ada_b': nrm(22, (DEPTH, 6 * D_MODEL), 0.01),
        'ln_g': 1.0 + nrm(23, (DEPTH, 2, D_MODEL), 0.01),
        'ln_b': nrm(24, (DEPTH, 2, D_MODEL), 0.01),
        'ffn_w_gate': nrm(25, (N_DENSE, D_MODEL, D_FF), D_MODEL ** -0.5),
        'ffn_w_up': nrm(26, (N_DENSE, D_MODEL, D_FF), D_MODEL ** -0.5),
        'ffn_w_down': nrm(27, (N_DENSE, D_FF, D_MODEL), DEEPNORM_BETA * D_FF ** -0.5),
        'moe_router': nrm(28, (N_MOE, D_MODEL, N_EXPERTS), D_MODEL ** -0.5),
        'moe_w_gate': nrm(29, (N_MOE, N_EXPERTS, D_MODEL, D_FF), D_MODEL ** -0.5),
        'moe_w_up': nrm(30, (N_MOE, N_EXPERTS, D_MODEL, D_FF), D_MODEL ** -0.5),
        'moe_w_down': nrm(31, (N_MOE, N_EXPERTS, D_FF, D_MODEL), DEEPNORM_BETA * D_FF ** -0.5),
    }


def reference(x, c, w_in, cmp_pos_k, cmp_pos_v, cmp_w1_k, cmp_w2_k, cmp_w1_v, cmp_w2_v,
              ssm_a_re, ssm_a_im, ssm_log_dt, ssm_b_re, ssm_b_im, ssm_c_re, ssm_c_im, ssm_d,
              ssm_w_glu, norm_attn, norm_ssm, w_out, ada_w, ada_b, ln_g, ln_b,
              ffn_w_gate, ffn_w_up, ffn_w_down, moe_router, moe_w_gate, moe_w_up, moe_w_down):
    c_act = jax.nn.silu(c)
    for l in range(DEPTH):
        mod = (c_act @ ada_w[l] + ada_b[l])[:, None, :]
        sh1, sc1, g1, sh2, sc2, g2 = jnp.split(mod, 6, axis=-1)
        y = hybrid_mixer(x * (1.0 + sc1) + sh1, w_in[l], cmp_pos_k[l], cmp_pos_v[l],
                         cmp_w1_k[l], cmp_w2_k[l], cmp_w1_v[l], cmp_w2_v[l],
                         ssm_a_re[l], ssm_a_im[l], ssm_log_dt[l], ssm_b_re[l], ssm_b_im[l],
                         ssm_c_re[l], ssm_c_im[l], ssm_d[l], ssm_w_glu[l],
                         norm_attn[l], norm_ssm[l], w_out[l])
        x = layer_norm(DEEPNORM_ALPHA * x + (1.0 + g1) * y, ln_g[l, 0], ln_b[l, 0])
        h = x * (1.0 + sc2) + sh2
        if l % 2 == 0:
            f = swiglu(h, ffn_w_gate[l // 2], ffn_w_up[l // 2], ffn_w_down[l // 2])
        else:
            f = moe_swiglu(h, moe_router[l // 2], moe_w_gate[l // 2], moe_w_up[l // 2], moe_w_down[l // 2])
        x = layer_norm(DEEPNORM_ALPHA * x + (1.0 + g2) * f, ln_g[l, 1], ln_b[l, 1])
    return x
```

```python
import numpy as np
import concourse.bass as bass
import concourse.mybir as mybir
from concourse.bass_utils import run_bass_kernel_spmd
from contextlib import ExitStack

F32 = mybir.dt.float32
BF16 = mybir.dt.bfloat16
I32 = mybir.dt.int32
AF = mybir.ActivationFunctionType
ALU = mybir.AluOpType
AX = mybir.AxisListType

EP = 30000
NDSEM = 40
NCSEM = 48

L = 4096
D = 1024
NT = 32
DFF = 2816
NFT = 22
DEPTH = 4
ALPHA = (2.0 * DEPTH) ** 0.25
LN_EPS = 1e-5
RMS_EPS = 1e-6
PROJ = 1816
C_Q, C_KC, C_VC, C_KS, C_VS, C_KW, C_VW, C_G, C_U = 0, 512, 640, 768, 896, 1024, 1152, 1280, 1304


class Buf:
    __slots__ = ("name", "w", "r", "excl")

    def __init__(self, name="", excl=False):
        self.name = name
        self.excl = excl
        self.w = None
        self.r = []


class Prog:
    ENG = ["pe", "act", "dve", "pool", "sp"]

    def __init__(self, nc, st):
        self.nc = nc
        self.st = st
        self.seq = {e: 0 for e in self.ENG}
        self.known = {e: {} for e in self.ENG}
        self.snaps = {e: [None] for e in self.ENG}
        self.esems = {e: [] for e in self.ENG}
        self.dsems = [st.enter_context(nc.semaphore("dq%d" % i)) for i in range(NDSEM)]
        self.dcount = 0
        self.csems = [st.enter_context(nc.semaphore("cq%d" % i)) for i in range(NCSEM)]
        self.ccount = 0
        self.dsnap = {}
        self.nwaits = 0
        self.engobj = {"pe": nc.tensor, "act": nc.scalar, "dve": nc.vector, "pool": nc.gpsimd, "sp": nc.sync}

    def _dsem(self, si):
        return self.dsems[si] if si < NDSEM else self.csems[si - NDSEM]

    def _esem(self, e, epoch):
        lst = self.esems[e]
        while len(lst) <= epoch:
            lst.append(self.st.enter_context(self.nc.semaphore("s_%s%d" % (e, len(lst)))))
        return lst[epoch]

    def _need(self, eng, ev, waits):
        if ev is None:
            return
        kn = self.known[eng]
        if ev[0] == "e":
            _, e2, n = ev
            if e2 == eng and eng == "pe":
                return
            key = ("e", e2)
            if kn.get(key, 0) >= n:
                return
            waits.append(ev)
            snap = self.snaps[e2][n]
            for k, v in snap.items():
                if kn.get(k, 0) < v:
                    kn[k] = v
            kn[key] = n
        else:
            _, si, val = ev
            key = ("d", si)
            if kn.get(key, 0) >= val:
                return
            waits.append(ev)
            kn[key] = val
            snap = self.dsnap.get((si, val))
            if snap:
                for k, v in snap.items():
                    if kn.get(k, 0) < v:
                        kn[k] = v

    def _deps(self, eng, reads, writes):
        waits = []
        for b in reads:
            self._need(eng, b.w, waits)
            if b.excl:
                for ev in b.r:
                    if not (ev[0] == "e" and ev[1] == eng):
                        self._need(eng, ev, waits)
        for b in writes:
            self._need(eng, b.w, waits)
            for ev in b.r:
                self._need(eng, ev, waits)
        return waits

    def op(self, eng, fn, reads=(), writes=()):
        waits = self._deps(eng, reads, writes)
        self.seq[eng] += 1
        n = self.seq[eng]
        ev = ("e", eng, n)
        self.snaps[eng].append(dict(self.known[eng]))
        for b in reads:
            b.r.append(ev)
        for b in writes:
            b.w = ev
            b.r = []
        self._emit(eng, waits, fn, ("e", n))
        return ev

    def dma(self, eng, fn, reads=(), writes=(), ring=0):
        if ring == 0:
            k = self.dcount
            self.dcount += 1
            si = k % NDSEM
            val = 16 * (k // NDSEM + 1)
            full = k >= NDSEM
        else:
            k = self.ccount
            self.ccount += 1
            si = NDSEM + k % NCSEM
            val = 16 * (k // NCSEM + 1)
            full = k >= NCSEM
        waits = self._deps(eng, reads, writes)
        if full:
            self._need(eng, ("d", si, val - 16), waits)
        ev = ("d", si, val)
        self.dsnap[(si, val)] = dict(self.known[eng])
        for b in reads:
            b.r.append(ev)
        for b in writes:
            b.w = ev
            b.r = []
        self._emit(eng, waits, fn, ("d", si))
        return ev

    def wait_all(self, eng, bufs):
        waits = []
        for b in bufs:
            self._need(eng, b.w, waits)
        self._emit(eng, waits, None, None)

    def barrier(self):
        for e in self.ENG:
            waits = []
            for e2 in self.ENG:
                if self.seq[e2]:
                    self._need(e, ("e", e2, self.seq[e2]), waits)
            for k in range(max(0, self.dcount - NDSEM), self.dcount):
                self._need(e, ("d", k % NDSEM, 16 * (k // NDSEM + 1)), waits)
            self._emit(e, waits, None, None)

    def _emit(self, e, waits, fn, kind):
        engine = self.engobj[e]
        for ev in waits:
            self.nwaits += 1
            if ev[0] == "e":
                _, e2, n = ev
                engine.wait_ge(self._esem(e2, (n - 1) // EP), (n - 1) % EP + 1)
            else:
                engine.wait_ge(self._dsem(ev[1]), ev[2])
        if fn is None:
            return
        inst = fn(engine)
        if kind[0] == "e":
            n = kind[1]
            inst.then_inc(self._esem(e, (n - 1) // EP), 1)
        else:
            inst.then_inc(self._dsem(kind[1]), 16)


class Rot:
    def __init__(self, mk, scope, name, shape, dt, n):
        self.items = [mk.T(scope, "%s%d" % (name, i), shape, dt) for i in range(n)]
        self.i = 0

    def next(self):
        it = self.items[self.i % len(self.items)]
        self.i += 1
        return it


WSPEC = {
    "x": [L, D], "c": [D], "w_in": [4, D, PROJ],
    "cmp_pos_k": [4, 32, 64], "cmp_pos_v": [4, 32, 64], "cmp_w1_k": [4, 32, 64, 64], "cmp_w2_k": [4, 64, 64],
    "cmp_w1_v": [4, 32, 64, 64], "cmp_w2_v": [4, 64, 64],
    "ssm_a_re": [4, 32, 64], "ssm_a_im": [4, 32, 64], "ssm_log_dt": [4, 32],
    "ssm_b_re": [4, 32, 64, 16], "ssm_b_im": [4, 32, 64, 16], "ssm_c_re": [4, 32, 16, 64], "ssm_c_im": [4, 32, 16, 64],
    "ssm_d": [4, 32, 16], "ssm_w_glu": [4, 512, 512], "norm_attn": [4, 512], "norm_ssm": [4, 512],
    "w_out": [4, D, D], "ada_w": [4, D, 6 * D], "ada_b": [4, 6 * D], "ln_g": [4, 2, D], "ln_b": [4, 2, D],
    "ffn_w_gate": [2, D, DFF], "ffn_w_up": [2, D, DFF], "ffn_w_down": [2, DFF, D],
    "moe_router": [2, D, 8], "moe_w_gate": [2, 8, D, DFF], "moe_w_up": [2, 8, D, DFF], "moe_w_down": [2, 8, DFF, D],
}


class MK:
    def __init__(self, dbg=None, phases=None):
        self.dbg = dbg or {}
        self.phases = phases
        self.nc = bass.Bass("TRN2", target_bir_lowering=False)
        self.I = {}
        nc = self.nc
        for k, shp in WSPEC.items():
            self.I[k] = nc.dram_tensor(k, shp, F32, kind="ExternalInput").ap()
        self.out = nc.dram_tensor("out", [L, D], F32, kind="ExternalOutput").ap()
        self.b_out = [Buf("out%d" % i) for i in range(NT)]
        self.b_xs = [Buf("xs%d" % i) for i in range(NT)]
        self.S = {}
        self.SB = {}

    def scratch(self, name, shape, dt):
        kind = "ExternalOutput" if name in self.dbg else "Internal"
        self.S[name] = self.nc.dram_tensor(name, shape, dt, kind=kind).ap()
        self.SB[name] = Buf(name)
        return self.S[name]

    def T(self, scope, name, shape, dt):
        self.tcount = getattr(self, "tcount", 0) + 1
        t = scope.enter_context(self.nc.sbuf_tensor("sb%d_%s" % (self.tcount, name), shape, dt))
        return t, Buf(name)

    def build(self):
        nc = self.nc
        with ExitStack() as st:
            self.p = p = Prog(nc, st)
            self.st = st
            self.ps = []
            for i in range(8):
                t = st.enter_context(nc.psum_tensor("ps%d" % i, [128, 512], F32))
                self.ps.append((t, Buf("ps%d" % i, excl=True)))
            self.psi = 0
            self.consts()
            self.scratch("xs", [L, D], F32)
            self.scratch("modrow", [4, 6 * D], F32)
            self.scratch("mixT", [D, L], BF16)
            self.scratch("uT", [512, L], BF16)
            self.scratch("ysT", [512, L], F32)
            for nm, shp, dt in (("kcmpT", [64, 2, 256], BF16), ("RCv", [128, 2, 2, 66], BF16), ("gsig", [128, NT, 24], F32),
                                ("seldbg", [NT, 2, 128, 64], F32), ("oattn", [L, 512], F32), ("mixdbg", [L, D], F32)):
                if nm in self.dbg:
                    self.scratch(nm, shp, dt)
            layers = list(self.dbg.get("layers", range(DEPTH)))
            self.cast_weights(layers[:1])
            self.emit_casts()
            self.ada_phase()
            p.barrier()
            for l in layers:
                src = self.I["x"] if l == layers[0] else self.S["xs"]
                srcb = [Buf("xin%d" % i) for i in range(NT)] if l == layers[0] else self.b_xs
                if l != layers[-1] and not (self.dbg.get("layers") is None and l >= 1):
                    self.cast_weights([layers[layers.index(l) + 1]])
                if self.phases is None or "mix" in self.phases:
                    self.mixer_phase(l, src, srcb)
                    p.barrier()
                    src, srcb = self.S["xs"], self.b_xs
                if self.phases is None or "ffn" in self.phases:
                    last = (l == layers[-1])
                    self.emit_casts()
                    self.ffn_phase(l, src, srcb, self.out if last else self.S["xs"], self.b_out if last else self.b_xs)
                    p.barrier()

            p.wait_all("sp", self.b_out + self.b_xs + [self.SB[k] for k in self.SB])
        return nc

    def bank(self):
        it = self.ps[self.psi % 8]
        self.psi += 1
        return it

    def consts(self):
        p, st = self.p, self.st
        self.reg0 = self.nc.gpsimd.to_reg(0.0)
        self.regp1 = self.nc.gpsimd.to_reg(1.0)
        self.regm1 = self.nc.gpsimd.to_reg(-1.0)
        self.ident, self.b_ident = self.T(st, "ident", [128, 128], F32)
        ident = self.ident
        p.op("pool", lambda e: e.memset(ident[:], 0.0), writes=[self.b_ident])
        p.op("pool", lambda e: e.affine_select(out=ident[:], in_=ident[:], pattern=[[-1, 128]], compare_op=ALU.not_equal,
                                               fill=self.regp1, base=0, channel_multiplier=1), reads=[self.b_ident], writes=[self.b_ident])
        self.ones, self.b_ones = self.T(st, "ones", [128, 128], F32)
        ones = self.ones
        p.op("pool", lambda e: e.memset(ones[:], 1.0), writes=[self.b_ones])
        self.modT, self.b_modT = self.T(st, "modT", [128, 4, 4, 8], F32)

    def emit_casts(self, n=None):
        k = len(self.pending) if n is None else min(n, len(self.pending))
        for fn in self.pending[:k]:
            fn()
        del self.pending[:k]

    def cast_weights(self, layers):
        p, I = self.p, self.I
        if not hasattr(self, "pending"):
            self.pending = []

        def cast(name, out_ap, in_ap):
            b = self.SB[name]
            self.pending.append(lambda: p.dma("pool", lambda e: e.dma_start(out=out_ap, in_=in_ap), writes=[b], ring=1))

        for l in layers:
            if self.phases is None or "mix" in self.phases:
                a = self.scratch("w_in_b%d" % l, [D, PROJ], BF16)
                cast("w_in_b%d" % l, a, I["w_in"][l])
                a = self.scratch("w_out_b%d" % l, [D, D], BF16)
                cast("w_out_b%d" % l, a, I["w_out"][l])
                a = self.scratch("w_glu_b%d" % l, [512, 512], BF16)
                cast("w_glu_b%d" % l, a, I["ssm_w_glu"][l])
            if not (self.phases is None or "ffn" in self.phases):
                continue
            li = l // 2
            srcs = [(I["ffn_w_gate"][li], I["ffn_w_up"][li], I["ffn_w_down"][li])] if l % 2 == 0 else \
                [(I["moe_w_gate"][li, e], I["moe_w_up"][li, e], I["moe_w_down"][li, e]) for e in range(8)]
            for e, (wg, wu, wd) in enumerate(srcs):
                for nm, w in (("g", wg), ("u", wu)):
                    name = "w%s_b%d_%d" % (nm, l, e)
                    a = self.scratch(name, [11, D, 256], BF16)
                    for fc in range(11):
                        cast(name, a[fc], w[:, fc * 256:(fc + 1) * 256])
                name = "wd_b%d_%d" % (l, e)
                a = self.scratch(name, [DFF, D], BF16)
                for h in range(2):
                    cast(name, a[h * 1408:(h + 1) * 1408, :], wd[h * 1408:(h + 1) * 1408, :])

    def ada_phase(self):
        p, I, nc = self.p, self.I, self.nc
        with ExitStack() as ph:
            cT, b_cT = self.T(ph, "cT", [128, 8], F32)
            ce, b_ce = self.T(ph, "ce", [128, 8], F32)
            p.dma("sp", lambda e: e.dma_start(out=cT[:], in_=I["c"].rearrange("(k p) -> p k", p=128), allow_slow_non_contiguous=True), writes=[b_cT])
            p.op("act", lambda e: e.activation(out=ce[:], in_=cT[:], func=AF.Exp, scale=-1.0), reads=[b_cT], writes=[b_ce])
            p.op("dve", lambda e: e.tensor_scalar_add(out=ce[:], in0=ce[:], scalar1=1.0), reads=[b_ce], writes=[b_ce])
            p.op("dve", lambda e: e.reciprocal(out=ce[:], in_=ce[:]), reads=[b_ce], writes=[b_ce])
            p.op("dve", lambda e: e.tensor_tensor(out=cT[:], in0=cT[:], in1=ce[:], op=ALU.mult), reads=[b_ce, b_cT], writes=[b_cT])
            wrot = Rot(self, ph, "adaw", [128, 8, 1024], F32, 2)
            brot = Rot(self, ph, "adab", [1, 1024], F32, 2)
            rrot = Rot(self, ph, "adar", [1, 1024], F32, 2)
            for l in self.dbg.get("layers", range(DEPTH)):
                for s in range(6):
                    wt, b_wt = wrot.next()
                    bt, b_bt = brot.next()
                    rt, b_rt = rrot.next()
                    p.dma("sp", lambda e, wt=wt, l=l, s=s: e.dma_start(out=wt[:], in_=I["ada_w"][l][:, s * 1024:(s + 1) * 1024].rearrange("(kt p) n -> p kt n", p=128)), writes=[b_wt])
                    p.dma("sp", lambda e, bt=bt, l=l, s=s: e.dma_start(out=bt[:], in_=I["ada_b"][l:l + 1, s * 1024:(s + 1) * 1024]), writes=[b_bt])
                    for hf in range(2):
                        pt, b_pt = self.bank()
                        for kt in range(8):
                            p.op("pe", lambda e, pt=pt, kt=kt, wt=wt, hf=hf: e.matmul(pt[0:1, :], lhsT=cT[:, kt:kt + 1], rhs=wt[:, kt, hf * 512:(hf + 1) * 512], start=(kt == 0), stop=(kt == 7)),
                                 reads=[b_cT, b_wt], writes=[b_pt])
                        addc = 1.0 if s in (1, 2, 4, 5) else 0.0
                        p.op("dve", lambda e, pt=pt, bt=bt, rt=rt, hf=hf, addc=addc: e.scalar_tensor_tensor(out=rt[:, hf * 512:(hf + 1) * 512], in0=pt[0:1, :], scalar=addc, in1=bt[:, hf * 512:(hf + 1) * 512], op0=ALU.add, op1=ALU.add),
                             reads=[b_pt, b_bt], writes=[b_rt])
                    p.dma("sp", lambda e, rt=rt, l=l, s=s: e.dma_start(out=self.S["modrow"][l:l + 1, s * 1024:(s + 1) * 1024], in_=rt[:]), reads=[b_rt], writes=[self.SB["modrow"]])
                    if s in (0, 1, 3, 4):
                        si = {0: 0, 1: 1, 3: 2, 4: 3}[s]
                        pt, b_pt = self.bank()
                        for j in range(8):
                            p.op("pe", lambda e, pt=pt, rt=rt, j=j: e.matmul(pt[:, j:j + 1], lhsT=rt[0:1, j * 128:(j + 1) * 128], rhs=self.ones[0:1, 0:1], start=True, stop=True),
                                 reads=[b_rt, self.b_ones], writes=[b_pt])
                        p.op("dve", lambda e, pt=pt, l=l, si=si: e.tensor_copy(out=self.modT[:, l, si, :], in_=pt[:, 0:8]), reads=[b_pt], writes=[self.b_modT])

    def load_bcast(self, eng, dst, b_dst, src_row, reads=()):
        self.p.dma(eng, lambda e: e.dma_start(out=dst, in_=src_row.partition_broadcast(128)), reads=list(reads), writes=[b_dst])

    def ln_tmps(self, ph, n=4):
        self.eps_ln, self.b_eps = self.T(ph, "epsln", [128, 1], F32)
        self.p.op("pool", lambda e: e.memset(self.eps_ln[:], LN_EPS), writes=[self.b_eps])
        slots = []
        for i in range(n):
            st6, b_st6 = self.T(ph, "st6_%d" % i, [128, 2, 6], F32)
            mv, b_mv = self.T(ph, "mv_%d" % i, [128, 2], F32)
            rs, b_rs = self.T(ph, "rs_%d" % i, [128, 1], F32)
            slots.append((st6, b_st6, mv, b_mv, rs, b_rs))
        self.ln_i = 0
        return slots

    def ln_pre(self, z, b_z, slots):
        p = self.p
        slot = slots[self.ln_i % len(slots)]
        self.ln_i += 1
        st6, b_st6, mv, b_mv, rs, b_rs = slot
        for hf in range(2):
            p.op("dve", lambda e, hf=hf: e.bn_stats(out=st6[:, hf, :], in_=z[:, hf * 512:(hf + 1) * 512]), reads=[b_z], writes=[b_st6])
        p.op("dve", lambda e: e.bn_aggr(out=mv[:], in_=st6[:]), reads=[b_st6], writes=[b_mv])
        p.op("act", lambda e: e.activation(out=rs[:], in_=mv[:, 1:2], func=AF.Ln, bias=self.eps_ln[:], scale=1.0), reads=[b_mv, self.b_eps], writes=[b_rs])
        p.op("act", lambda e: e.activation(out=rs[:], in_=rs[:], func=AF.Exp, scale=-0.5), reads=[b_rs], writes=[b_rs])
        return slot

    def ln_post(self, z, b_z, slot, lng, b_lng, lnb, b_lnb, outt, b_out):
        p = self.p
        st6, b_st6, mv, b_mv, rs, b_rs = slot
        p.op("dve", lambda e: e.scalar_tensor_tensor(out=z[:], in0=z[:], scalar=mv[:, 0:1], in1=lng[:], op0=ALU.subtract, op1=ALU.mult), reads=[b_z, b_mv, b_lng], writes=[b_z])
        p.op("dve", lambda e: e.scalar_tensor_tensor(out=outt[:], in0=z[:], scalar=rs[:, 0:1], in1=lnb[:], op0=ALU.mult, op1=ALU.add), reads=[b_z, b_rs, b_lnb], writes=[b_out])

    def transpose_mod(self, xt, b_xt, dst_fn, b_dst, l, si_sh, si_sc):
        p = self.p
        for hf in range(2):
            pt, b_pt = self.bank()
            for j in range(4):
                kt = hf * 4 + j
                p.op("pe", lambda e, pt=pt, j=j, kt=kt: e.transpose(out=pt[:, j * 128:(j + 1) * 128], in_=xt[:, kt * 128:(kt + 1) * 128], identity=self.ident[:]),
                     reads=[b_xt, self.b_ident], writes=[b_pt])
            for j in range(4):
                kt = hf * 4 + j
                p.op("act", lambda e, pt=pt, j=j, kt=kt: e.activation(out=dst_fn(kt), in_=pt[:, j * 128:(j + 1) * 128], func=AF.Identity,
                                                                   scale=self.modT[:, l, si_sc, kt:kt + 1], bias=self.modT[:, l, si_sh, kt:kt + 1]),
                     reads=[b_pt, self.b_modT], writes=[b_dst])

    def ffn_phase(self, l, src, b_src, dst, b_dst):
        p, I, S = self.p, self.I, self.S
        if l == 1 and self.dbg.get("layers") is None:
            self.cast_weights([2, 3])
        moe = (l % 2 == 1)
        li = l // 2
        E = 8 if moe else 1
        with ExitStack() as ph:
            hT, _ = self.T(ph, "hT", [128, 8, 1024], BF16)
            b_hT = [Buf("hT%d" % i) for i in range(8)]
            acc, _ = self.T(ph, "acc", [128, 8, 1024], F32)
            b_acc = [Buf("acc%d" % i) for i in range(8)]
            act, _ = self.T(ph, "actT", [128, NFT, 1024], BF16)
            b_act = [Buf("act%d" % i) for i in range(NFT)]
            wd, _ = self.T(ph, "wd", [128, NFT, 1024], BF16)
            b_wd = [Buf("wd0"), Buf("wd1")]
            wgrot = Rot(self, ph, "wg", [128, 8, 256], BF16, 2)
            wurot = Rot(self, ph, "wu", [128, 8, 256], BF16, 2)
            xrot = Rot(self, ph, "xin", [128, 1024], F32, 3)
            zrot = Rot(self, ph, "zt", [128, 1024], F32, 3)
            srot = Rot(self, ph, "silu", [128, 512], BF16, 3)
            gB, b_gB = self.T(ph, "gB", [128, 1024], F32)
            lng, b_lng = self.T(ph, "lng", [128, 1024], F32)
            lnb, b_lnb = self.T(ph, "lnb", [128, 1024], F32)
            tmp = self.ln_tmps(ph)
            self.load_bcast("sp", gB[:], b_gB, S["modrow"][l:l + 1, 5 * 1024:6 * 1024], reads=[self.SB["modrow"]])
            self.load_bcast("sp", lng[:], b_lng, I["ln_g"][l, 1:2, :])
            self.load_bcast("sp", lnb[:], b_lnb, I["ln_b"][l, 1:2, :])
            if moe:
                rt, b_rt = self.T(ph, "rt", [128, 8, 8], F32)
                p.dma("sp", lambda e: e.dma_start(out=rt[:], in_=I["moe_router"][li].rearrange("(kt p) e -> p kt e", p=128)), writes=[b_rt])
                gate, b_gate = self.T(ph, "gate", [128, 2, 8, 8], F32)
                h32rot = Rot(self, ph, "h32", [128, 8, 128], F32, 2)
                lg, b_lg = self.T(ph, "lg", [128, 8], F32)
                srt, b_srt = self.T(ph, "srt", [128, 8], F32)
                gw, b_gw = self.T(ph, "gw", [128, 4], F32)
                g2t, b_g2t = self.T(ph, "g2t", [128, 8], F32)
            def F0(tg):
                for tt in range(8):
                    t0 = tg * 1024 + tt * 128
                    xt, b_xt = xrot.next()
                    p.dma("sp", lambda e, xt=xt, t0=t0: e.dma_start(out=xt[:], in_=src[t0:t0 + 128, :]), reads=[b_src[tg * 8 + tt]], writes=[b_xt])
                    if not moe:
                        self.transpose_mod(xt, b_xt, lambda kt, tt=tt: hT[:, kt, tt * 128:(tt + 1) * 128], b_hT[tt], l, 2, 3)
                    else:
                        h32, b_h32 = h32rot.next()
                        self.transpose_mod(xt, b_xt, lambda kt, h32=h32: h32[:, kt, :], b_h32, l, 2, 3)
                        pt, b_pt = self.bank()
                        for kt in range(8):
                            p.op("pe", lambda e, pt=pt, kt=kt, h32=h32: e.matmul(pt[:, 0:8], lhsT=h32[:, kt, :], rhs=rt[:, kt, :], start=(kt == 0), stop=(kt == 7)),
                                 reads=[b_h32, b_rt], writes=[b_pt])
                        p.op("dve", lambda e, h32=h32, tt=tt: e.tensor_copy(out=hT[:, :, tt * 128:(tt + 1) * 128], in_=h32[:]), reads=[b_h32], writes=[b_hT[tt]])
                        p.op("dve", lambda e, pt=pt: e.tensor_copy(out=lg[:], in_=pt[:, 0:8]), reads=[b_pt], writes=[b_lg])
                        p.op("dve", lambda e: e.max(out=srt[:], in_=lg[:]), reads=[b_lg], writes=[b_srt])
                        p.op("dve", lambda e: e.tensor_tensor(out=gw[:, 0:1], in0=srt[:, 1:2], in1=srt[:, 0:1], op=ALU.subtract), reads=[b_srt], writes=[b_gw])
                        p.op("act", lambda e: e.activation(out=gw[:, 1:2], in_=gw[:, 0:1], func=AF.Exp), reads=[b_gw], writes=[b_gw])
                        p.op("dve", lambda e: e.tensor_scalar_add(out=gw[:, 2:3], in0=gw[:, 1:2], scalar1=1.0), reads=[b_gw], writes=[b_gw])
                        p.op("dve", lambda e: e.reciprocal(out=gw[:, 2:3], in_=gw[:, 2:3]), reads=[b_gw], writes=[b_gw])
                        p.op("dve", lambda e: e.tensor_tensor(out=gw[:, 3:4], in0=gw[:, 1:2], in1=gw[:, 2:3], op=ALU.mult), reads=[b_gw], writes=[b_gw])
                        p.op("dve", lambda e: e.tensor_scalar(out=g2t[:], in0=lg[:], scalar1=srt[:, 1:2], scalar2=gw[:, 3:4], op0=ALU.is_equal, op1=ALU.mult),
                             reads=[b_lg, b_srt, b_gw], writes=[b_g2t])
                        p.op("dve", lambda e, tt=tt: e.tensor_scalar(out=gate[:, tg % 2, tt, :], in0=lg[:], scalar1=srt[:, 0:1], scalar2=gw[:, 2:3], op0=ALU.is_equal, op1=ALU.mult),
                             reads=[b_lg, b_srt, b_gw], writes=[b_gate])
                        p.op("dve", lambda e, tt=tt: e.tensor_tensor(out=gate[:, tg % 2, tt, :], in0=gate[:, tg % 2, tt, :], in1=g2t[:], op=ALU.add), reads=[b_g2t, b_gate], writes=[b_gate])
            F0(0)
            for tg in range(4):
                def f2_pre(tt):
                    t0 = tg * 1024 + tt * 128
                    xt, b_xt = xrot.next()
                    zt, b_zt = zrot.next()
                    p.dma("sp", lambda e: e.dma_start(out=xt[:], in_=src[t0:t0 + 128, :]), reads=[b_src[tg * 8 + tt]], writes=[b_xt])
                    p.op("dve", lambda e: e.tensor_tensor(out=zt[:], in0=acc[:, tt, :], in1=gB[:], op=ALU.mult), reads=[b_acc[tt], b_gB], writes=[b_zt])
                    p.op("dve", lambda e: e.scalar_tensor_tensor(out=zt[:], in0=xt[:], scalar=ALPHA, in1=zt[:], op0=ALU.mult, op1=ALU.add), reads=[b_xt, b_zt], writes=[b_zt])
                    return xt, b_xt, zt, b_zt, self.ln_pre(zt, b_zt, tmp)

                def f2_post(tt, c):
                    t0 = tg * 1024 + tt * 128
                    xt, b_xt, zt, b_zt, slot = c
                    self.ln_post(zt, b_zt, slot, lng, b_lng, lnb, b_lnb, xt, b_xt)
                    p.dma("sp", lambda e: e.dma_start(out=dst[t0:t0 + 128, :], in_=xt[:]), reads=[b_xt], writes=[b_dst[tg * 8 + tt]])

                cs_ = {}
                for ex in range(E):
                    wdn = "wd_b%d_%d" % (l, ex)
                    for h in range(2):
                        p.dma("sp", lambda e, wdn=wdn, h=h: e.dma_start(out=wd[:, h * 11:(h + 1) * 11, :], in_=S[wdn][h * 1408:(h + 1) * 1408, :].rearrange("(ft p) n -> p ft n", p=128)),
                              reads=[self.SB[wdn]], writes=[b_wd[h]])
                    for fc in range(11):
                        self.emit_casts(1)
                        wg, b_wg = wgrot.next()
                        wu, b_wu = wurot.next()
                        gn, un = "wg_b%d_%d" % (l, ex), "wu_b%d_%d" % (l, ex)
                        p.dma("sp", lambda e, wg=wg, gn=gn, fc=fc: e.dma_start(out=wg[:], in_=S[gn][fc].rearrange("(kt p) f -> p kt f", p=128)), reads=[self.SB[gn]], writes=[b_wg])
                        p.dma("sp", lambda e, wu=wu, un=un, fc=fc: e.dma_start(out=wu[:], in_=S[un][fc].rearrange("(kt p) f -> p kt f", p=128)), reads=[self.SB[un]], writes=[b_wu])
                        for f2 in range(2):
                            ft = fc * 2 + f2
                            for tc in range(2):
                                pg, b_pg = self.bank()
                                pu, b_pu = self.bank()
                                hb = b_hT[tc * 4:(tc + 1) * 4]
                                for kt in range(8):
                                    p.op("pe", lambda e, pg=pg, wg=wg, kt=kt, f2=f2, tc=tc: e.matmul(pg[:], lhsT=wg[:, kt, f2 * 128:(f2 + 1) * 128], rhs=hT[:, kt, tc * 512:(tc + 1) * 512], start=(kt == 0), stop=(kt == 7)),
                                         reads=[b_wg] + hb, writes=[b_pg])
                                for kt in range(8):
                                    p.op("pe", lambda e, pu=pu, wu=wu, kt=kt, f2=f2, tc=tc: e.matmul(pu[:], lhsT=wu[:, kt, f2 * 128:(f2 + 1) * 128], rhs=hT[:, kt, tc * 512:(tc + 1) * 512], start=(kt == 0), stop=(kt == 7)),
                                         reads=[b_wu] + hb, writes=[b_pu])
                                sl, b_sl = srot.next()
                                p.op("act", lambda e, sl=sl, pg=pg: e.activation(out=sl[:], in_=pg[:], func=AF.Silu), reads=[b_pg], writes=[b_sl])
                                p.op("dve", lambda e, sl=sl, pu=pu, ft=ft, tc=tc: e.tensor_tensor(out=act[:, ft, tc * 512:(tc + 1) * 512], in0=sl[:], in1=pu[:], op=ALU.mult),
                                     reads=[b_sl, b_pu], writes=[b_act[ft]])
                    if ex == E - 1 and tg + 1 < 4:
                        F0(tg + 1)
                    for tt in range(8):
                        for hf in range(2):
                            pd, b_pd = self.bank()
                            for ft in range(NFT):
                                p.op("pe", lambda e, pd=pd, ft=ft, tt=tt, hf=hf: e.matmul(pd[:], lhsT=act[:, ft, tt * 128:(tt + 1) * 128], rhs=wd[:, ft, hf * 512:(hf + 1) * 512], start=(ft == 0), stop=(ft == NFT - 1)),
                                     reads=[b_act[ft], b_wd[ft // 11]], writes=[b_pd])
                            a_sl = acc[:, tt, hf * 512:(hf + 1) * 512]
                            if not moe:
                                p.op("dve", lambda e, pd=pd, a_sl=a_sl: e.tensor_copy(out=a_sl, in_=pd[:]), reads=[b_pd], writes=[b_acc[tt]])
                            elif ex == 0:
                                p.op("dve", lambda e, pd=pd, a_sl=a_sl, tt=tt, ex=ex: e.tensor_scalar(out=a_sl, in0=pd[:], scalar1=gate[:, tg % 2, tt, ex:ex + 1], scalar2=None, op0=ALU.mult),
                                     reads=[b_pd, b_gate], writes=[b_acc[tt]])
                            else:
                                p.op("dve", lambda e, pd=pd, a_sl=a_sl, tt=tt, ex=ex: e.scalar_tensor_tensor(out=a_sl, in0=pd[:], scalar=gate[:, tg % 2, tt, ex:ex + 1], in1=a_sl, op0=ALU.mult, op1=ALU.add),
                                     reads=[b_pd, b_gate, b_acc[tt]], writes=[b_acc[tt]])
                        if ex == E - 1:
                            cs_[tt] = f2_pre(tt)
                            if tt >= 2:
                                f2_post(tt - 2, cs_.pop(tt - 2))
                    if ex == E - 1:
                        f2_post(6, cs_.pop(6))
                        f2_post(7, cs_.pop(7))

    def pb(self, i):
        return self.ps[i]

    def evac(self, k, out_ap, in_ap, reads, writes):
        if k % 2 == 0:
            self.p.op("act", lambda e: e.activation(out=out_ap, in_=in_ap, func=AF.Copy), reads=reads, writes=writes)
        else:
            self.p.op("dve", lambda e: e.tensor_copy(out=out_ap, in_=in_ap), reads=reads, writes=writes)

    def mixer_phase(self, l, src, b_src):
        sub = self.dbg.get("mixsub", ("attn", "ssm", "glu", "out"))
        self.attn_cast_n = 2 if (self.dbg.get("layers") is None and self.phases is None) else 7
        if "attn" in sub:
            self.mix_attn(l, src, b_src)
            self.p.barrier()
        if "ssm" in sub:
            self.mix_ssm(l)
            self.p.barrier()
        if "glu" in sub:
            self.mix_glu(l)
            self.p.barrier()
        if "out" in sub:
            self.mix_out(l, src, b_src)

    def mix_attn(self, l, src, b_src):
        p, I, S = self.p, self.I, self.S
        with ExitStack() as ph:
            T = lambda name, shape, dt: self.T(ph, name, shape, dt)
            qT, _ = T("qT", [64, 8, L], BF16)
            b_qT = [Buf("qT%d" % i) for i in range(8)]
            KX, _ = T("KX", [128, 2, L], BF16)
            b_KX = [Buf("KX%d" % i) for i in range(8)]
            b_KXc = Buf("KXc")
            kwT, _ = T("kwT", [64, 2, L], BF16)
            b_kwT = [Buf("kwT%d" % i) for i in range(8)]
            kcT, b_kcT = T("kcT", [128, L], BF16)
            vcT, b_vcT = T("vcT", [128, L], BF16)
            vsA, _ = T("vsA", [128, NT, 2, 66], BF16)
            b_vsA = [Buf("vsA%d" % i) for i in range(NT)]
            vwA, _ = T("vwA", [128, NT, 2, 66], BF16)
            b_vwA = [Buf("vwA%d" % i) for i in range(NT)]
            b_one = Buf("vones")
            gsig, _ = T("gsig", [128, NT, 24], F32)
            b_gs = [Buf("gs%d" % i) for i in range(NT)]
            kcmpT, b_kcmpT = T("kcmpT", [64, 2, 256], BF16)
            RCv, b_RCv = T("RCv", [128, 2, 2, 66], BF16)
            ov, b_ov = T("ov", [128, 2, 64], BF16)
            p.op("pool", lambda e: e.memset(KX[64:128], 1.0), writes=[b_KXc])
            p.op("pool", lambda e: e.affine_select(out=KX[64:128], in_=KX[64:128], pattern=[[0, 2], [1, L]], compare_op=ALU.is_ge, fill=self.reg0, base=0, channel_multiplier=-64), reads=[b_KXc], writes=[b_KXc])
            p.op("pool", lambda e: e.affine_select(out=KX[64:128], in_=KX[64:128], pattern=[[0, 2], [-1, L]], compare_op=ALU.is_ge, fill=self.reg0, base=63, channel_multiplier=64), reads=[b_KXc], writes=[b_KXc])
            p.op("pool", lambda e: e.memset(vsA[:, :, :, 64:65], 1.0), writes=[b_one])
            p.op("pool", lambda e: e.memset(vwA[:, :, :, 64:65], 1.0), writes=[b_one])
            p.op("pool", lambda e: e.memset(RCv[:], 0.0), writes=[b_RCv])
            p.op("pool", lambda e: e.memset(RCv[:, :, :, 64:65], 1.0), reads=[b_RCv], writes=[b_RCv])
            p.op("pool", lambda e: e.memset(ov[:], 1.0), writes=[b_ov])
            for nt in range(2):
                p.op("pool", lambda e, nt=nt: e.affine_select(out=ov[:, nt, :], in_=ov[:, nt, :], pattern=[[-4, 64]], compare_op=ALU.is_ge, fill=self.reg0, base=1 + 128 * nt, channel_multiplier=1), reads=[b_ov], writes=[b_ov])
                p.op("pool", lambda e, nt=nt: e.affine_select(out=ov[:, nt, :], in_=ov[:, nt, :], pattern=[[4, 64]], compare_op=ALU.is_ge, fill=self.reg0, base=3 - 128 * nt, channel_multiplier=-1), reads=[b_ov], writes=[b_ov])
            with ExitStack() as p1:
                win, b_win = self.T(p1, "win", [128, 8, PROJ], BF16)
                wn = "w_in_b%d" % l
                p.dma("sp", lambda e: e.dma_start(out=win[:], in_=S[wn].rearrange("(kt p) n -> p kt n", p=128)), reads=[self.SB[wn]], writes=[b_win])
                hrot = Rot(self, p1, "hTc", [128, 8, 512], BF16, 2)
                xrot = Rot(self, p1, "xin", [128, 1024], F32, 2)
                urot = Rot(self, p1, "uTc", [128, 4, 512], BF16, 2)
                ek = 0
                for tc in range(8):
                    hTc, b_hTc = hrot.next()
                    csl = slice(tc * 512, (tc + 1) * 512)
                    for tt in range(4):
                        t0 = tc * 512 + tt * 128
                        xt, b_xt = xrot.next()
                        p.dma("sp", lambda e, xt=xt, t0=t0: e.dma_start(out=xt[:], in_=src[t0:t0 + 128, :]), reads=[b_src[tc * 4 + tt]], writes=[b_xt])
                        self.transpose_mod(xt, b_xt, lambda kt, tt=tt, hTc=hTc: hTc[:, kt, tt * 128:(tt + 1) * 128], b_hTc, l, 0, 1)
                    uTc, b_uTc = urot.next()
                    groups = []
                    for hd in range(8):
                        groups.append((C_Q + 64 * hd, 64, qT[:, hd, csl], b_qT[tc]))
                    for h in range(2):
                        groups.append((C_KS + 64 * h, 64, KX[0:64, h, csl], b_KX[tc]))
                        groups.append((C_KW + 64 * h, 64, kwT[:, h, csl], b_kwT[tc]))
                    groups.append((C_KC, 128, kcT[:, csl], b_kcT))
                    groups.append((C_VC, 128, vcT[:, csl], b_vcT))
                    for ct in range(4):
                        groups.append((C_U + 128 * ct, 128, uTc[:, ct, :], b_uTc))
                    if self.dbg.get("m1lvl", 9) < 1:
                        groups = []
                    if self.dbg.get("m1lvl", 9) == 1:
                        groups = groups[:8]
                    if self.dbg.get("m1lvl", 9) == 2:
                        groups = groups[:14]
                    for gi, (c0, wdt, dst, b_d) in enumerate(groups):
                        pt, b_pt = self.pb(gi % 4)
                        for kt in range(8):
                            p.op("pe", lambda e, pt=pt, kt=kt, c0=c0, wdt=wdt, hTc=hTc: e.matmul(pt[0:wdt, :], lhsT=win[:, kt, c0:c0 + wdt], rhs=hTc[:, kt, :], start=(kt == 0), stop=(kt == 7)),
                                 reads=[b_win, b_hTc], writes=[b_pt])
                        self.evac(ek, dst, pt[0:wdt, :], [b_pt], [b_d])
                        ek += 1
                    p.dma("sp", lambda e, uTc=uTc, csl=csl: e.dma_start(out=S["uT"][:, csl].rearrange("(ct p) t -> p ct t", p=128), in_=uTc[:]), reads=[b_uTc], writes=[self.SB["uT"]])
                    for tt in range(4 if self.dbg.get("m1lvl", 9) > 3 else 0):
                        TT = tc * 4 + tt
                        pt, b_pt = self.pb(4 + tt % 2)
                        for kt in range(8):
                            p.op("pe", lambda e, pt=pt, kt=kt, tt=tt, hTc=hTc: e.matmul(pt[:, 0:408], lhsT=hTc[:, kt, tt * 128:(tt + 1) * 128], rhs=win[:, kt, C_VS:C_VS + 408], start=(kt == 0), stop=(kt == 7)),
                                 reads=[b_win, b_hTc], writes=[b_pt])
                        if self.dbg.get("m1lvl", 9) >= 5:
                            p.op("act", lambda e, pt=pt, TT=TT: e.activation(out=vsA[:, TT, :, 0:64], in_=pt[:, 0:128].rearrange("p (h d) -> p h d", h=2), func=AF.Copy), reads=[b_pt], writes=[b_vsA[TT]])
                        if self.dbg.get("m1lvl", 9) >= 6:
                            p.op("dve", lambda e, pt=pt, TT=TT: e.tensor_copy(out=vwA[:, TT, :, 0:64], in_=pt[:, 256:384].rearrange("p (h d) -> p h d", h=2)), reads=[b_pt], writes=[b_vwA[TT]])
                        if self.dbg.get("m1lvl", 9) >= 7:
                            p.op("act", lambda e, pt=pt, TT=TT: e.activation(out=gsig[:, TT, :], in_=pt[:, 384:408], func=AF.Exp, scale=-1.0), reads=[b_pt], writes=[b_gs[TT]])
                        if self.dbg.get("m1lvl", 9) >= 8:
                            p.op("dve", lambda e, TT=TT: e.tensor_scalar_add(out=gsig[:, TT, :], in0=gsig[:, TT, :], scalar1=1.0), reads=[b_gs[TT]], writes=[b_gs[TT]])
                        if self.dbg.get("m1lvl", 9) >= 8:
                            p.op("dve", lambda e, TT=TT: e.reciprocal(out=gsig[:, TT, :], in_=gsig[:, TT, :]), reads=[b_gs[TT]], writes=[b_gs[TT]])
            p.barrier()
            if self.dbg.get("attn_stop") == "m1":
                return
            with ExitStack() as p1:
                w1s, b_w1s = self.T(p1, "w1s", [128, 32, 64], F32)
                w1b, b_w1b = self.T(p1, "w1b", [128, 32, 64], BF16)
                w2s, b_w2s = self.T(p1, "w2s", [64, 64], F32)
                w2b, b_w2b = self.T(p1, "w2b", [64, 64], BF16)
                pss, b_pss = self.T(p1, "pss", [64, 32], F32)
                psb, b_psb = self.T(p1, "psb", [64, 32], BF16)
                cbias, b_cbias = self.T(p1, "cbias", [64, 1], F32)
                xh, b_xh = self.T(p1, "xh", [64, 256], F32)
                x2, b_x2 = self.T(p1, "x2", [64, 256], F32)
                hg, b_hg = self.T(p1, "hg", [64, 256], BF16)
                p.op("pool", lambda e: e.memset(hg[:], 0.0), writes=[b_hg])
                for kv, (w1n, w2n, posn, srcT, b_srcT) in enumerate((("cmp_w1_k", "cmp_w2_k", "cmp_pos_k", kcT, b_kcT), ("cmp_w1_v", "cmp_w2_v", "cmp_pos_v", vcT, b_vcT))):
                    for hh in range(2):
                        p.dma("sp", lambda e, hh=hh, w1n=w1n: e.dma_start(out=w1s[hh * 64:(hh + 1) * 64], in_=I[w1n][l].rearrange("l d f -> d l f")), writes=[b_w1s])
                    p.dma("sp", lambda e, w2n=w2n: e.dma_start(out=w2s[:], in_=I[w2n][l]), writes=[b_w2s])
                    p.dma("sp", lambda e, posn=posn: e.dma_start(out=pss[:], in_=I[posn][l].rearrange("l d -> d l"), allow_slow_non_contiguous=True), writes=[b_pss])
                    p.op("dve", lambda e: e.tensor_copy(out=w1b[:], in_=w1s[:]), reads=[b_w1s], writes=[b_w1b])
                    p.op("dve", lambda e: e.tensor_copy(out=w2b[:], in_=w2s[:]), reads=[b_w2s], writes=[b_w2b])
                    p.op("dve", lambda e: e.tensor_copy(out=psb[:], in_=pss[:]), reads=[b_pss], writes=[b_psb])
                    pt, b_pt = self.pb(0)
                    for ll in range(32):
                        p.op("pe", lambda e, pt=pt, ll=ll: e.matmul(pt[0:64, 0:1], lhsT=w1b[0:64, ll, :], rhs=psb[:, ll:ll + 1], start=(ll == 0), stop=(ll == 31)), reads=[b_w1b, b_psb], writes=[b_pt])
                    p.op("dve", lambda e, pt=pt: e.tensor_copy(out=cbias[:], in_=pt[0:64, 0:1]), reads=[b_pt], writes=[b_cbias])
                    for h in range(2):
                        pt, b_pt = self.pb(1 + h)
                        for ll in range(32):
                            p.op("pe", lambda e, pt=pt, ll=ll, h=h, srcT=srcT: e.matmul(pt[0:64, 0:255], lhsT=w1b[64 * h:64 * h + 64, ll, :], rhs=srcT[64 * h:64 * h + 64, ll:ll + 16 * 254 + 1:16], start=(ll == 0), stop=(ll == 31)),
                                 reads=[b_w1b, b_srcT], writes=[b_pt])
                        xv, x2v = xh[:, 0:255], x2[:, 0:255]
                        p.op("dve", lambda e, pt=pt: e.tensor_scalar(out=xv, in0=pt[0:64, 0:255], scalar1=cbias[:, 0:1], scalar2=None, op0=ALU.add), reads=[b_pt, b_cbias], writes=[b_xh])
                        p.op("dve", lambda e: e.tensor_tensor(out=x2v, in0=xv, in1=xv, op=ALU.mult), reads=[b_xh], writes=[b_x2])
                        p.op("dve", lambda e: e.tensor_scalar(out=x2v, in0=x2v, scalar1=0.044715, scalar2=1.0, op0=ALU.mult, op1=ALU.add), reads=[b_x2], writes=[b_x2])
                        p.op("dve", lambda e: e.tensor_tensor(out=x2v, in0=x2v, in1=xv, op=ALU.mult), reads=[b_x2, b_xh], writes=[b_x2])
                        p.op("act", lambda e: e.activation(out=x2v, in_=x2v, func=AF.Exp, scale=-1.5957691216), reads=[b_x2], writes=[b_x2])
                        p.op("dve", lambda e: e.tensor_scalar_add(out=x2v, in0=x2v, scalar1=1.0), reads=[b_x2], writes=[b_x2])
                        p.op("dve", lambda e: e.reciprocal(out=x2v, in_=x2v), reads=[b_x2], writes=[b_x2])
                        p.op("dve", lambda e: e.tensor_tensor(out=hg[:, 0:255], in0=x2v, in1=xv, op=ALU.mult), reads=[b_x2, b_xh], writes=[b_hg])
                        pt2, b_pt2 = self.pb(3 + h)
                        if kv == 0:
                            p.op("pe", lambda e, pt2=pt2: e.matmul(pt2[0:64, 0:256], lhsT=w2b[:], rhs=hg[:], start=True, stop=True), reads=[b_w2b, b_hg], writes=[b_pt2])
                            p.op("act", lambda e, pt2=pt2, h=h: e.activation(out=kcmpT[:, h, :], in_=pt2[0:64, 0:256], func=AF.Copy), reads=[b_pt2], writes=[b_kcmpT])
                        else:
                            for nt in range(2):
                                p.op("pe", lambda e, pt2=pt2, nt=nt: e.matmul(pt2[:, nt * 64:(nt + 1) * 64], lhsT=hg[:, nt * 128:(nt + 1) * 128], rhs=w2b[:], start=True, stop=True), reads=[b_w2b, b_hg], writes=[b_pt2])
                            p.op("act", lambda e, pt2=pt2, h=h: e.activation(out=RCv[:, :, h, 0:64], in_=pt2[:, 0:128].rearrange("p (n d) -> p n d", n=2), func=AF.Copy), reads=[b_pt2], writes=[b_RCv])
            p.barrier()
            if "kcmpT" in self.dbg:
                p.dma("sp", lambda e: e.dma_start(out=self.S["kcmpT"], in_=kcmpT[:]), reads=[b_kcmpT], writes=[self.SB["kcmpT"]])
                p.dma("sp", lambda e: e.dma_start(out=self.S["RCv"], in_=RCv[:]), reads=[b_RCv], writes=[self.SB["RCv"]])
                p.dma("sp", lambda e: e.dma_start(out=self.S["gsig"], in_=gsig[:]), reads=b_gs, writes=[self.SB["gsig"]])
            if self.dbg.get("attn_stop") == "m1b":
                return
            with ExitStack() as p2:
                T2 = lambda name, shape, dt: self.T(p2, name, shape, dt)
                erot = Rot(self, p2, "Et", [128, 512], BF16, 4)
                qxrot = Rot(self, p2, "QX", [128, 4, 128], BF16, 2)
                oat_rot = Rot(self, p2, "oat", [128, 512], F32, 2)
                selpad, b_selpad = T2("selpad", [128, 128], F32)
                imp, b_imp = T2("imp", [128, 64], F32)
                scb, b_scb = T2("scb", [128, 64], F32)
                m8, b_m8 = T2("m8", [128, 8], F32)
                thr, b_thr = T2("thr", [128, 1], F32)
                rd, b_rd = T2("rd", [128, 3, 4], F32)
                osb_rot = Rot(self, p2, "osb", [128, 4, 65], F32, 2)
                nat, b_nat = T2("nat", [128, 512], F32)
                ssq, b_ssq = T2("ssq", [128, 2], F32)
                junk, b_junk = T2("junk", [128, 512], F32)
                onT_rot = Rot(self, p2, "onT", [128, 4, 128], BF16, 2)
                epsr, b_epsr = T2("epsr", [128, 1], F32)
                p.op("pool", lambda e: e.memset(epsr[:], RMS_EPS), writes=[b_epsr])
                p.op("pool", lambda e: e.memset(selpad[:], 0.0), writes=[b_selpad])
                self.load_bcast("sp", nat[:], b_nat, I["norm_attn"][l:l + 1, :])
                sbi = [0]
                sc2, b_sc2 = T2("sc2", [128, 64], F32)
                SK = 2

                def run_tiles(tasks, hooks):
                    ctxs = []
                    for t in range(len(tasks) + SK):
                        if t < len(tasks):
                            ctxs.append(tasks[t][0]())
                        if t >= SK:
                            tasks[t - SK][1](ctxs[t - SK])
                            if (t - SK) in hooks:
                                hooks[t - SK]()

                oats = {}

                def part1(i, h):
                    if h == 0:
                        self.emit_casts(self.attn_cast_n)
                        oats[i] = oat_rot.next()
                    oat, b_oat = oats[i]
                    qsl = slice(i * 128, (i + 1) * 128)
                    qchunk = b_qT[i // 4]
                    Oc, b_Oc = self.pb(3)
                    IM, b_IM = self.pb(4)
                    Ow, b_Ow = self.pb(6)
                    qrhs = qT[:, 4 * h:4 * h + 4, qsl]
                    QX, b_QX = qxrot.next()

                    def topk():
                        p.op("dve", lambda e: e.tensor_scalar(out=rd[:, 0, :], in0=Oc[:, 0:260].rearrange("p (g d) -> p g d", g=4)[:, :, 64], scalar1=1e-30, scalar2=None, op0=ALU.max), reads=[b_Oc], writes=[b_rd])
                        p.op("dve", lambda e: e.reciprocal(out=rd[:, 0, :], in_=rd[:, 0, :]), reads=[b_rd], writes=[b_rd])
                        p.op("dve", lambda e: e.tensor_scalar(out=imp[:], in0=IM[:, 0:64], scalar1=rd[:, 0, 0:1], scalar2=None, op0=ALU.mult), reads=[b_IM, b_rd], writes=[b_imp])
                        for g in range(1, 4):
                            p.op("dve", lambda e, g=g: e.scalar_tensor_tensor(out=imp[:], in0=IM[:, g * 64:(g + 1) * 64], scalar=rd[:, 0, g:g + 1], in1=imp[:], op0=ALU.mult, op1=ALU.add), reads=[b_IM, b_rd, b_imp], writes=[b_imp])
                        for hq in range(2):
                            cur = 2 * i + hq
                            rows = slice(hq * 64, (hq + 1) * 64)
                            p.op("pool", lambda e, rows=rows, cur=cur: e.affine_select(out=scb[rows], in_=imp[rows], pattern=[[-2, 64]], compare_op=ALU.is_ge, fill=self.regm1, base=2 * cur + 1, channel_multiplier=0), reads=[b_imp], writes=[b_scb])
                            p.op("pool", lambda e, rows=rows: e.memset(scb[rows, 0:1], 1000.0), reads=[b_scb], writes=[b_scb])
                            if cur >= 1:
                                p.op("pool", lambda e, rows=rows, cur=cur: e.memset(scb[rows, cur:cur + 1], 1001.0), reads=[b_scb], writes=[b_scb])
                            if cur >= 2:
                                p.op("pool", lambda e, rows=rows, cur=cur: e.memset(scb[rows, cur - 1:cur], 1002.0), reads=[b_scb], writes=[b_scb])
                        p.op("dve", lambda e: e.max(out=m8[:], in_=scb[:]), reads=[b_scb], writes=[b_m8])
                        p.op("dve", lambda e: e.match_replace(out=sc2[:], in_to_replace=m8[:], in_values=scb[:], imm_value=-1e9), reads=[b_scb, b_m8], writes=[b_sc2])
                        p.op("dve", lambda e: e.max(out=m8[:], in_=sc2[:]), reads=[b_sc2], writes=[b_m8])
                        p.op("dve", lambda e: e.tensor_scalar(out=thr[:], in0=m8[:, 7:8], scalar1=-0.5, scalar2=None, op0=ALU.max), reads=[b_m8], writes=[b_thr])
                        p.op("dve", lambda e: e.tensor_scalar(out=selpad[:, 64:128], in0=scb[:], scalar1=thr[:, 0:1], scalar2=1.0, op0=ALU.is_ge, op1=ALU.subtract), reads=[b_scb, b_thr], writes=[b_selpad])
                        if "seldbg" in self.dbg:
                            p.dma("sp", lambda e: e.dma_start(out=self.S["seldbg"][i, h], in_=selpad[:, 64:128]), reads=[b_selpad], writes=[self.SB["seldbg"]])
                        combine(i, h, oat, b_oat, 0, Oc, b_Oc, True)

                    tasks = []
                    nts = [0] if 8 * i + 6 < 128 else [0, 1]
                    for nt in nts:
                        tasks.append((s1(kcmpT[:, h, nt * 128:(nt + 1) * 128], qrhs, [b_kcmpT, qchunk], (128 * i - 31 - 2048 * nt, -16, [[0, 4], [1, 128]])),
                                      s2([(Oc, b_Oc, 65, RCv[:, nt, h, 0:65], [b_RCv]), (IM, b_IM, 64, ov[:, nt, :], [b_ov])], nt == 0, nt == nts[-1])))
                    hooks = {len(nts) - 1: topk}
                    kts = list(range(max(0, i - 4), i + 1))
                    for kt in kts:
                        mask = None
                        if kt == i:
                            mask = (0, -1, [[0, 4], [1, 128]])
                        elif kt == i - 4:
                            mask = (-1, 1, [[0, 4], [-1, 128]])
                        tasks.append((s1(kwT[:, h, kt * 128:(kt + 1) * 128], qrhs, [b_kwT[kt // 4], qchunk], mask),
                                      s2([(Ow, b_Ow, 65, vwA[:, kt, h, 0:65], [b_vwA[kt], b_one])], kt == kts[0], kt == kts[-1])))
                    p.op("dve", lambda e: e.tensor_copy(out=QX[0:64], in_=qrhs), reads=[qchunk], writes=[b_QX])
                    run_tiles(tasks, hooks)
                    combine(i, h, oat, b_oat, 2, Ow, b_Ow, False)
                    pm, b_pm = self.pb(7)
                    p.op("pe", lambda e: e.transpose(out=pm[:, 0:128], in_=selpad[:], identity=self.ident[:]), reads=[b_selpad, self.b_ident], writes=[b_pm])
                    p.op("act", lambda e: e.activation(out=QX[64:128], in_=pm[64:128, 0:128].unsqueeze(1).to_broadcast([64, 4, 128]), func=AF.Copy, scale=30000.0), reads=[b_pm], writes=[b_QX])
                    return QX, b_QX

                def part2(i, h, ctx):
                    QX, b_QX = ctx
                    oat, b_oat = oats[i]
                    Os, b_Os = self.pb(5)
                    tasks = []
                    for kt in range(i + 1):
                        mask = (0, -1, [[0, 4], [1, 128]]) if kt == i else None
                        tasks.append((s1(KX[:, h, kt * 128:(kt + 1) * 128], QX[:].rearrange("p g q -> p (g q)"), [b_KX[kt // 4], b_KXc, b_QX], mask),
                                      s2([(Os, b_Os, 65, vsA[:, kt, h, 0:65], [b_vsA[kt], b_one])], kt == 0, kt == i)))
                    run_tiles(tasks, {})
                    combine(i, h, oat, b_oat, 1, Os, b_Os, False)

                def finalize(i):
                    oat, b_oat = oats.pop(i)
                    qsl = slice(i * 128, (i + 1) * 128)
                    if "oattn" in self.dbg:
                        p.dma("sp", lambda e: e.dma_start(out=self.S["oattn"][qsl, :], in_=oat[:]), reads=[b_oat], writes=[self.SB["oattn"]])
                    p.op("act", lambda e: e.activation(out=junk[:], in_=oat[:], func=AF.Square, accum_out=ssq[:, 0:1]), reads=[b_oat], writes=[b_junk, b_ssq])
                    p.op("act", lambda e: e.activation(out=ssq[:, 1:2], in_=ssq[:, 0:1], func=AF.Ln, scale=1.0 / 512, bias=epsr[:]), reads=[b_ssq, b_epsr], writes=[b_ssq])
                    p.op("act", lambda e: e.activation(out=ssq[:, 1:2], in_=ssq[:, 1:2], func=AF.Exp, scale=-0.5), reads=[b_ssq], writes=[b_ssq])
                    p.op("dve", lambda e: e.scalar_tensor_tensor(out=oat[:], in0=oat[:], scalar=ssq[:, 1:2], in1=nat[:], op0=ALU.mult, op1=ALU.mult), reads=[b_oat, b_ssq, b_nat], writes=[b_oat])
                    pm, b_pm = self.pb(7)
                    for j in range(4):
                        p.op("pe", lambda e, j=j: e.transpose(out=pm[:, j * 128:(j + 1) * 128], in_=oat[:, j * 128:(j + 1) * 128], identity=self.ident[:]), reads=[b_oat, self.b_ident], writes=[b_pm])
                    onT, b_onT = onT_rot.next()
                    p.op("dve", lambda e: e.tensor_copy(out=onT[:], in_=pm[:].rearrange("p (j q) -> p j q", j=4)), reads=[b_pm], writes=[b_onT])
                    p.dma("sp", lambda e: e.dma_start(out=S["mixT"][0:512, qsl].rearrange("(j p) t -> p j t", p=128), in_=onT[:]), reads=[b_onT], writes=[self.SB["mixT"]])

                def s1(lhsT, rhs, reads, mask):
                    def f():
                        pt, b_pt = self.pb(sbi[0] % 3)
                        sbi[0] += 1
                        p.op("pe", lambda e: e.matmul(pt[:], lhsT=lhsT, rhs=rhs, start=True, stop=True), reads=reads, writes=[b_pt])
                        Et, b_Et = erot.next()
                        p.op("act", lambda e: e.activation(out=Et[:], in_=pt[:], func=AF.Exp, scale=0.125), reads=[b_pt], writes=[b_Et])
                        if mask is not None:
                            base, cm, pat = mask
                            Ev = Et[:].rearrange("p (g q) -> p g q", g=4)
                            p.op("pool", lambda e: e.affine_select(out=Ev, in_=Ev, pattern=pat, compare_op=ALU.is_ge, fill=self.reg0, base=base, channel_multiplier=cm), reads=[b_Et], writes=[b_Et])
                        return Et, b_Et
                    return f

                def s2(accs, first, last):
                    def f(ctx):
                        Et, b_Et = ctx
                        for acc, b_acc, width, rhs, reads in accs:
                            for g in range(4):
                                p.op("pe", lambda e, g=g: e.matmul(acc[:, g * width:(g + 1) * width], lhsT=Et[:, g * 128:(g + 1) * 128], rhs=rhs, start=(first and g == 0), stop=last, skip_group_check=True),
                                     reads=[b_Et] + reads, writes=[b_acc])
                    return f

                def combine(i, h, oat, b_oat, b, acc, b_acc, first):
                    av = acc[:, 0:260].rearrange("p (g d) -> p g d", g=4)
                    if b > 0:
                        p.op("dve", lambda e: e.tensor_scalar(out=rd[:, b, :], in0=av[:, :, 64], scalar1=1e-30, scalar2=None, op0=ALU.max), reads=[b_acc], writes=[b_rd])
                        p.op("dve", lambda e: e.reciprocal(out=rd[:, b, :], in_=rd[:, b, :]), reads=[b_rd], writes=[b_rd])
                    gv = gsig[:, i, h * 12:(h + 1) * 12].rearrange("p (g b) -> p g b", b=3)[:, :, b]
                    p.op("dve", lambda e: e.tensor_tensor(out=rd[:, b, :], in0=rd[:, b, :], in1=gv, op=ALU.mult), reads=[b_rd, b_gs[i]], writes=[b_rd])
                    for g in range(4):
                        osl = oat[:, h * 256 + g * 64: h * 256 + (g + 1) * 64]
                        if first:
                            p.op("dve", lambda e, g=g, osl=osl: e.tensor_scalar(out=osl, in0=av[:, g, 0:64], scalar1=rd[:, b, g:g + 1], scalar2=None, op0=ALU.mult), reads=[b_acc, b_rd], writes=[b_oat])
                        else:
                            p.op("dve", lambda e, g=g, osl=osl: e.scalar_tensor_tensor(out=osl, in0=av[:, g, 0:64], scalar=rd[:, b, g:g + 1], in1=osl, op0=ALU.mult, op1=ALU.add), reads=[b_acc, b_rd, b_oat], writes=[b_oat])

                seqs = [(i, h) for i in range(self.dbg.get("attn_nblk", NT)) for h in range(2)]
                ctx = part1(*seqs[0])
                for k, (i, h) in enumerate(seqs):
                    nxt = part1(*seqs[k + 1]) if k + 1 < len(seqs) else None
                    part2(i, h, ctx)
                    if h == 1:
                        finalize(i)
                    ctx = nxt
                if self.attn_cast_n == 7:
                    self.emit_casts()

    def mix_ssm(self, l):
        p, I, S = self.p, self.I, self.S
        MAGIC = 12582912.0
        TWO_PI_S = 6.28318
        with ExitStack() as ph:
            T = lambda name, shape, dt: self.T(ph, name, shape, dt)
            rr, b_rr = T("rr", [128, 32], F32)
            th, b_th = T("th", [128, 32], F32)
            cst, b_cst = T("cst", [128, 5], F32)
            TB1, b_TB1 = T("TB1", [128, 4, 128], F32)
            TB2, b_TB2 = T("TB2", [128, 4, 128], F32)
            TC1, b_TC1 = T("TC1", [128, 4, 128], F32)
            TC2, b_TC2 = T("TC2", [128, 4, 128], F32)
            rowm, b_rowm = T("rowm", [128, 8], F32)
            colm, b_colm = T("colm", [128, 8, 128], F32)
            dT, b_dT = T("dT", [128, 4], F32)
            inner = ExitStack()
            Ti = lambda name, shape, dt: self.T(inner, name, shape, dt)
            are, b_are = Ti("are", [128, 32], F32)
            aim, b_aim = Ti("aim", [128, 32], F32)
            dt_, b_dt = Ti("dt", [128, 32], F32)
            cs, b_cs = Ti("cs", [128, 2, 32], F32)
            t1, b_t1 = Ti("t1", [128, 32], F32)
            t2, b_t2 = Ti("t2", [128, 32], F32)
            kr, b_kr = Ti("kr", [128, 32], F32)
            ki, b_ki = Ti("ki", [128, 32], F32)
            for hh in range(2):
                p.dma("sp", lambda e, hh=hh: e.dma_start(out=are[hh * 64:(hh + 1) * 64], in_=I["ssm_a_re"][l].rearrange("g p -> p g"), allow_slow_non_contiguous=True), writes=[b_are])
                p.dma("sp", lambda e, hh=hh: e.dma_start(out=aim[hh * 64:(hh + 1) * 64], in_=I["ssm_a_im"][l].rearrange("g p -> p g"), allow_slow_non_contiguous=True), writes=[b_aim])
            self.load_bcast("sp", dt_[:], b_dt, I["ssm_log_dt"][l:l + 1, :])
            p.op("act", lambda e: e.activation(out=dt_[:], in_=dt_[:], func=AF.Exp), reads=[b_dt], writes=[b_dt])
            p.op("dve", lambda e: e.tensor_tensor(out=rr[:], in0=are[:], in1=dt_[:], op=ALU.mult), reads=[b_are, b_dt], writes=[b_rr])
            p.op("act", lambda e: e.activation(out=rr[:], in_=rr[:], func=AF.Exp), reads=[b_rr], writes=[b_rr])
            p.op("dve", lambda e: e.tensor_tensor(out=th[:], in0=aim[:], in1=dt_[:], op=ALU.mult), reads=[b_aim, b_dt], writes=[b_th])
            p.op("dve", lambda e: e.tensor_scalar(out=th[:], in0=th[:], scalar1=1.0 / (2.0 * np.pi), scalar2=None, op0=ALU.mult), reads=[b_th], writes=[b_th])

            for ci, cv in enumerate((0.25, 0.0, MAGIC, -MAGIC, 1.570795)):
                p.op("pool", lambda e, ci=ci, cv=cv: e.memset(cst[:, ci:ci + 1], cv), reads=[b_cst], writes=[b_cst])

            def sincos(out_ap, b_o, v_ap, b_v, off, tmpa, b_ta, tmpb, b_tb, n_eng="dve", mul=1.0, b_mul=None):
                oc = 0 if off == 0.25 else 1
                p.op("act", lambda e: e.activation(out=tmpa, in_=v_ap, func=AF.Identity, scale=mul, bias=cst[:, oc:oc + 1]), reads=[b_v, b_cst] + ([b_mul] if b_mul else []), writes=[b_ta])
                p.op("act", lambda e: e.activation(out=tmpb, in_=tmpa, func=AF.Identity, scale=1.0, bias=cst[:, 2:3]), reads=[b_ta, b_cst], writes=[b_tb])
                p.op("act", lambda e: e.activation(out=tmpb, in_=tmpb, func=AF.Identity, scale=1.0, bias=cst[:, 3:4]), reads=[b_tb, b_cst], writes=[b_tb])
                p.op(n_eng, lambda e: e.tensor_tensor(out=tmpb, in0=tmpb, in1=tmpa, op=ALU.subtract), reads=[b_ta, b_tb], writes=[b_tb])
                p.op("act", lambda e: e.activation(out=out_ap, in_=tmpb, func=AF.Sin, scale=-TWO_PI_S), reads=[b_tb], writes=[b_o])

            sincos(cs[:, 0, :], b_cs, th[:], b_th, 0.25, t1[:], b_t1, t2[:], b_t2)
            sincos(cs[:, 1, :], b_cs, th[:], b_th, 0.0, t1[:], b_t1, t2[:], b_t2)
            nr, b_nr = Ti("nr", [128, 32], F32)
            ni, b_ni = Ti("ni", [128, 32], F32)
            p.op("dve", lambda e: e.tensor_tensor(out=nr[:], in0=rr[:], in1=cs[:, 0, :], op=ALU.mult), reads=[b_rr, b_cs], writes=[b_nr])
            p.op("dve", lambda e: e.tensor_scalar_add(out=nr[:], in0=nr[:], scalar1=-1.0), reads=[b_nr], writes=[b_nr])
            p.op("dve", lambda e: e.tensor_tensor(out=ni[:], in0=rr[:], in1=cs[:, 1, :], op=ALU.mult), reads=[b_rr, b_cs], writes=[b_ni])
            p.op("dve", lambda e: e.tensor_tensor(out=t1[:], in0=are[:], in1=are[:], op=ALU.mult), reads=[b_are], writes=[b_t1])
            p.op("dve", lambda e: e.tensor_tensor(out=t2[:], in0=aim[:], in1=aim[:], op=ALU.mult), reads=[b_aim], writes=[b_t2])
            p.op("dve", lambda e: e.tensor_tensor(out=t1[:], in0=t1[:], in1=t2[:], op=ALU.add), reads=[b_t1, b_t2], writes=[b_t1])
            p.op("dve", lambda e: e.reciprocal(out=t1[:], in_=t1[:]), reads=[b_t1], writes=[b_t1])
            p.op("dve", lambda e: e.tensor_tensor(out=kr[:], in0=nr[:], in1=are[:], op=ALU.mult), reads=[b_nr, b_are], writes=[b_kr])
            p.op("dve", lambda e: e.tensor_tensor(out=t2[:], in0=ni[:], in1=aim[:], op=ALU.mult), reads=[b_ni, b_aim], writes=[b_t2])
            p.op("dve", lambda e: e.tensor_tensor(out=kr[:], in0=kr[:], in1=t2[:], op=ALU.add), reads=[b_kr, b_t2], writes=[b_kr])
            p.op("dve", lambda e: e.tensor_tensor(out=kr[:], in0=kr[:], in1=t1[:], op=ALU.mult), reads=[b_kr, b_t1], writes=[b_kr])
            p.op("dve", lambda e: e.tensor_tensor(out=ki[:], in0=ni[:], in1=are[:], op=ALU.mult), reads=[b_ni, b_are], writes=[b_ki])
            p.op("dve", lambda e: e.tensor_tensor(out=t2[:], in0=nr[:], in1=aim[:], op=ALU.mult), reads=[b_nr, b_aim], writes=[b_t2])
            p.op("dve", lambda e: e.tensor_tensor(out=ki[:], in0=ki[:], in1=t2[:], op=ALU.subtract), reads=[b_ki, b_t2], writes=[b_ki])
            p.op("dve", lambda e: e.tensor_tensor(out=ki[:], in0=ki[:], in1=t1[:], op=ALU.mult), reads=[b_ki, b_t1], writes=[b_ki])
            BR, b_BR = Ti("BR", [64, 32, 16], F32)
            BI, b_BI = Ti("BI", [64, 32, 16], F32)
            Bpr, b_Bpr = Ti("Bpr", [64, 32, 16], F32)
            Bpi, b_Bpi = Ti("Bpi", [64, 32, 16], F32)
            Btmp, b_Btmp = Ti("Btmp", [64, 32, 16], F32)
            p.dma("sp", lambda e: e.dma_start(out=BR[:], in_=I["ssm_b_re"][l].rearrange("g p h -> p g h")), writes=[b_BR])
            p.dma("sp", lambda e: e.dma_start(out=BI[:], in_=I["ssm_b_im"][l].rearrange("g p h -> p g h")), writes=[b_BI])
            krb = kr[0:64, :].unsqueeze(2).to_broadcast([64, 32, 16])
            kib = ki[0:64, :].unsqueeze(2).to_broadcast([64, 32, 16])
            p.op("dve", lambda e: e.tensor_tensor(out=Bpr[:], in0=BR[:], in1=krb, op=ALU.mult), reads=[b_BR, b_kr], writes=[b_Bpr])
            p.op("dve", lambda e: e.tensor_tensor(out=Btmp[:], in0=BI[:], in1=kib, op=ALU.mult), reads=[b_BI, b_ki], writes=[b_Btmp])
            p.op("dve", lambda e: e.tensor_tensor(out=Bpr[:], in0=Bpr[:], in1=Btmp[:], op=ALU.subtract), reads=[b_Bpr, b_Btmp], writes=[b_Bpr])
            p.op("dve", lambda e: e.tensor_tensor(out=Bpi[:], in0=BI[:], in1=krb, op=ALU.mult), reads=[b_BI, b_kr], writes=[b_Bpi])
            p.op("dve", lambda e: e.tensor_tensor(out=Btmp[:], in0=BR[:], in1=kib, op=ALU.mult), reads=[b_BR, b_ki], writes=[b_Btmp])
            p.op("dve", lambda e: e.tensor_tensor(out=Bpi[:], in0=Bpi[:], in1=Btmp[:], op=ALU.add), reads=[b_Bpi, b_Btmp], writes=[b_Bpi])
            for ct in range(4):
                pr, b_pr = self.pb(0 + ct % 2)
                pi_, b_pi = self.pb(2 + ct % 2)
                p.op("pe", lambda e, ct=ct, pr=pr: e.transpose(out=pr[:, 0:64], in_=Bpr[:, 8 * ct:8 * ct + 8, :].rearrange("p g h -> p (g h)"), identity=self.ident[0:64, 0:64]), reads=[b_Bpr, self.b_ident], writes=[b_pr])
                p.op("pe", lambda e, ct=ct, pi_=pi_: e.transpose(out=pi_[:, 0:64], in_=Bpi[:, 8 * ct:8 * ct + 8, :].rearrange("p g h -> p (g h)"), identity=self.ident[0:64, 0:64]), reads=[b_Bpi, self.b_ident], writes=[b_pi])
                p.op("dve", lambda e, ct=ct, pr=pr: e.tensor_copy(out=TB1[:, ct, 0:64], in_=pr[:, 0:64]), reads=[b_pr], writes=[b_TB1])
                p.op("dve", lambda e, ct=ct, pi_=pi_: e.tensor_copy(out=TB1[:, ct, 64:128], in_=pi_[:, 0:64]), reads=[b_pi], writes=[b_TB1])
                p.op("dve", lambda e, ct=ct, pi_=pi_: e.tensor_copy(out=TB2[:, ct, 0:64], in_=pi_[:, 0:64]), reads=[b_pi], writes=[b_TB2])
                p.op("dve", lambda e, ct=ct, pr=pr: e.tensor_scalar(out=TB2[:, ct, 64:128], in0=pr[:, 0:64], scalar1=-1.0, scalar2=None, op0=ALU.mult), reads=[b_pr], writes=[b_TB2])
            CR, b_CR = Ti("CR", [128, 4, 64], F32)
            CI, b_CI = Ti("CI", [128, 4, 64], F32)
            Cc1, b_Cc1 = Ti("Cc1", [128, 4, 128], F32)
            Cc2, b_Cc2 = Ti("Cc2", [128, 4, 128], F32)
            p.dma("sp", lambda e: e.dma_start(out=CR[:], in_=I["ssm_c_re"][l].rearrange("(ct g) h p -> (g h) ct p", g=8)), writes=[b_CR])
            p.dma("sp", lambda e: e.dma_start(out=CI[:], in_=I["ssm_c_im"][l].rearrange("(ct g) h p -> (g h) ct p", g=8)), writes=[b_CI])
            p.op("dve", lambda e: e.tensor_copy(out=Cc1[:, :, 0:64], in_=CR[:]), reads=[b_CR], writes=[b_Cc1])
            p.op("dve", lambda e: e.tensor_scalar(out=Cc1[:, :, 64:128], in0=CI[:], scalar1=-1.0, scalar2=None, op0=ALU.mult), reads=[b_CI], writes=[b_Cc1])
            p.op("dve", lambda e: e.tensor_scalar(out=Cc2[:, :, 0:64], in0=CI[:], scalar1=-1.0, scalar2=None, op0=ALU.mult), reads=[b_CI], writes=[b_Cc2])
            p.op("dve", lambda e: e.tensor_scalar(out=Cc2[:, :, 64:128], in0=CR[:], scalar1=-1.0, scalar2=None, op0=ALU.mult), reads=[b_CR], writes=[b_Cc2])
            for ct in range(4):
                p1_, b_p1 = self.pb(4 + ct % 2)
                p2_, b_p2 = self.pb(6 + ct % 2)
                p.op("pe", lambda e, ct=ct, p1_=p1_: e.transpose(out=p1_[:, 0:128], in_=Cc1[:, ct, :], identity=self.ident[:]), reads=[b_Cc1, self.b_ident], writes=[b_p1])
                p.op("pe", lambda e, ct=ct, p2_=p2_: e.transpose(out=p2_[:, 0:128], in_=Cc2[:, ct, :], identity=self.ident[:]), reads=[b_Cc2, self.b_ident], writes=[b_p2])
                p.op("dve", lambda e, ct=ct, p1_=p1_: e.tensor_copy(out=TC1[:, ct, :], in_=p1_[:, 0:128]), reads=[b_p1], writes=[b_TC1])
                p.op("dve", lambda e, ct=ct, p2_=p2_: e.tensor_copy(out=TC2[:, ct, :], in_=p2_[:, 0:128]), reads=[b_p2], writes=[b_TC2])
            p.op("pool", lambda e: e.memset(rowm[:], 1.0), writes=[b_rowm])
            p.op("pool", lambda e: e.affine_select(out=rowm[:], in_=rowm[:], pattern=[[-16, 8]], compare_op=ALU.is_ge, fill=self.reg0, base=0, channel_multiplier=1), reads=[b_rowm], writes=[b_rowm])
            p.op("pool", lambda e: e.affine_select(out=rowm[:], in_=rowm[:], pattern=[[16, 8]], compare_op=ALU.is_ge, fill=self.reg0, base=15, channel_multiplier=-1), reads=[b_rowm], writes=[b_rowm])
            p.op("pool", lambda e: e.memset(colm[:], 0.0), writes=[b_colm])
            for g8 in range(8):
                p.op("pool", lambda e, g8=g8: e.memset(colm[:, g8, 16 * g8:16 * g8 + 16], 1.0), reads=[b_colm], writes=[b_colm])
            p.dma("sp", lambda e: e.dma_start(out=dT[:], in_=I["ssm_d"][l].rearrange("(ct g) h -> (g h) ct", g=8), allow_slow_non_contiguous=True), writes=[b_dT])
            p.barrier()
            inner.close()
            iot, b_iot = T("iot", [128, L], F32)
            p.op("pool", lambda e: e.iota(out=iot[:], pattern=[[1, L]], base=0, channel_multiplier=0, allow_small_or_imprecise_dtypes=True), writes=[b_iot])
            ones5, b_ones5 = T("ones5", [128, 512], F32)
            p.op("pool", lambda e: e.memset(ones5[:], 1.0), writes=[b_ones5])
            ta, b_ta = T("ta", [128, L], F32)
            tb, b_tb = T("tb", [128, L], F32)
            td, b_td = T("td", [128, L], F32)
            cosrot = Rot(self, ph, "COS", [128, L], F32, 2)
            sinrot = Rot(self, ph, "SIN", [128, L], F32, 2)
            urot = Rot(self, ph, "uTt", [128, L], BF16, 2)
            yacc, b_yacc = T("yacc", [128, L], F32)
            lrot = [Rot(self, ph, nm, [128, 128], BF16, 2) for nm in ("TB1g", "TB2g", "TC1g", "TC2g")]
            RMrot = Rot(self, ph, "RM", [128, 512], F32, 2)
            m1rot = Rot(self, ph, "m1", [128, 512], F32, 3)
            m2rot = Rot(self, ph, "m2", [128, 512], F32, 3)
            zrot = Rot(self, ph, "zs", [128, 512], F32, 5)
            xcrot = Rot(self, ph, "xc", [128, 512], BF16, 2)
            xsrot = Rot(self, ph, "xs_", [128, 512], BF16, 2)
            bk = [0]

            def gen_tables(g):
                COS, b_COS = cosrot.next()
                SIN, b_SIN = sinrot.next()
                p.op("act", lambda e: e.activation(out=ta[:], in_=iot[:], func=AF.Identity, scale=th[:, g:g + 1], bias=cst[:, 1:2]), reads=[b_iot, b_cst, b_th], writes=[b_ta])
                p.op("act", lambda e: e.activation(out=tb[:], in_=ta[:], func=AF.Identity, scale=1.0, bias=cst[:, 2:3]), reads=[b_ta, b_cst], writes=[b_tb])
                p.op("act", lambda e: e.activation(out=tb[:], in_=tb[:], func=AF.Identity, scale=1.0, bias=cst[:, 3:4]), reads=[b_tb, b_cst], writes=[b_tb])
                p.op("pool", lambda e: e.tensor_tensor(out=tb[:], in0=tb[:], in1=ta[:], op=ALU.subtract), reads=[b_ta, b_tb], writes=[b_tb])
                p.op("act", lambda e: e.activation(out=SIN[:], in_=tb[:], func=AF.Sin, scale=-TWO_PI_S), reads=[b_tb], writes=[b_SIN])
                p.op("act", lambda e: e.activation(out=td[:], in_=tb[:], func=AF.Abs), reads=[b_tb], writes=[b_td])
                p.op("act", lambda e: e.activation(out=COS[:], in_=td[:], func=AF.Sin, scale=-TWO_PI_S, bias=cst[:, 4:5]), reads=[b_td, b_cst], writes=[b_COS])
                return COS, b_COS, SIN, b_SIN

            tabs = {0: gen_tables(0)}
            for ct in range(4):
                uTt, b_uTt = urot.next()
                p.dma("sp", lambda e, uTt=uTt, ct=ct: e.dma_start(out=uTt[:], in_=S["uT"][ct * 128:(ct + 1) * 128, :]), reads=[self.SB["uT"]], writes=[b_uTt])
                for g8 in range(8):
                    g = ct * 8 + g8
                    COS, b_COS, SIN, b_SIN = tabs.pop(g)
                    self.emit_casts(2)
                    (TB1g, b_1), (TB2g, b_2), (TC1g, b_3), (TC2g, b_4) = [r_.next() for r_ in lrot]
                    p.op("dve", lambda e: e.tensor_scalar(out=TB1g[:], in0=TB1[:, ct, :], scalar1=rowm[:, g8:g8 + 1], scalar2=None, op0=ALU.mult), reads=[b_TB1, b_rowm], writes=[b_1])
                    p.op("dve", lambda e: e.tensor_scalar(out=TB2g[:], in0=TB2[:, ct, :], scalar1=rowm[:, g8:g8 + 1], scalar2=None, op0=ALU.mult), reads=[b_TB2, b_rowm], writes=[b_2])
                    p.op("dve", lambda e: e.tensor_tensor(out=TC1g[:], in0=TC1[:, ct, :], in1=colm[:, g8, :], op=ALU.mult), reads=[b_TC1, b_colm], writes=[b_3])
                    p.op("dve", lambda e: e.tensor_tensor(out=TC2g[:], in0=TC2[:, ct, :], in1=colm[:, g8, :], op=ALU.mult), reads=[b_TC2, b_colm], writes=[b_4])
                    RM, b_RM = RMrot.next()
                    p.op("dve", lambda e: e.tensor_scalar(out=RM[:], in0=ones5[:], scalar1=rr[:, g:g + 1], scalar2=None, op0=ALU.mult), reads=[b_ones5, b_rr], writes=[b_RM])
                    if g + 1 < 32:
                        tabs[g + 1] = gen_tables(g + 1)
                    zprev = [None]

                    def stage_a1(tc):
                        csl = slice(tc * 512, (tc + 1) * 512)
                        P1, b_P1 = self.pb(bk[0] % 2)
                        P2, b_P2 = self.pb(2 + bk[0] % 2)
                        bk[0] += 1
                        p.op("pe", lambda e: e.matmul(P1[:], lhsT=TB1g[:], rhs=uTt[:, csl], start=True, stop=True), reads=[b_1, b_uTt], writes=[b_P1])
                        p.op("pe", lambda e: e.matmul(P2[:], lhsT=TB2g[:], rhs=uTt[:, csl], start=True, stop=True), reads=[b_2, b_uTt], writes=[b_P2])
                        m1, b_m1 = m1rot.next()
                        m2, b_m2 = m2rot.next()
                        p.op("dve", lambda e: e.tensor_tensor(out=m1[:], in0=COS[:, csl], in1=P1[:], op=ALU.mult), reads=[b_COS, b_P1], writes=[b_m1])
                        p.op("dve", lambda e: e.tensor_tensor(out=m2[:], in0=SIN[:, csl], in1=P2[:], op=ALU.mult), reads=[b_SIN, b_P2], writes=[b_m2])
                        p.op("pool", lambda e: e.tensor_tensor(out=m1[:], in0=m1[:], in1=m2[:], op=ALU.add), reads=[b_m1, b_m2], writes=[b_m1])
                        return m1, b_m1

                    def stage_a2(tc, mm):
                        m1, b_m1 = mm
                        z, b_z = zrot.next()
                        init = 0.0 if zprev[0] is None else zprev[0][0][:, 511:512]
                        rds = [b_RM, b_m1] + ([] if zprev[0] is None else [zprev[0][1]])
                        p.op("dve", lambda e: e.tensor_tensor_scan(out=z[:], data0=RM[:], data1=m1[:], initial=init, op0=ALU.mult, op1=ALU.add), reads=rds, writes=[b_z])
                        zprev[0] = (z, b_z)
                        return z, b_z

                    def stage_b1(tc, zz):
                        z, b_z = zz
                        csl = slice(tc * 512, (tc + 1) * 512)
                        Y, b_Y = self.pb(4 + tc % 4)
                        xc, b_xc = xcrot.next()
                        xs_, b_xs = xsrot.next()
                        p.op("dve", lambda e: e.tensor_tensor(out=xc[:], in0=COS[:, csl], in1=z[:], op=ALU.mult), reads=[b_COS, b_z], writes=[b_xc])
                        p.op("pool", lambda e: e.tensor_tensor(out=xs_[:], in0=SIN[:, csl], in1=z[:], op=ALU.mult), reads=[b_SIN, b_z], writes=[b_xs])
                        p.op("pe", lambda e: e.matmul(Y[:], lhsT=TC1g[:], rhs=xc[:], start=True, stop=False), reads=[b_3, b_xc], writes=[b_Y])
                        p.op("pe", lambda e: e.matmul(Y[:], lhsT=TC2g[:], rhs=xs_[:], start=False, stop=True), reads=[b_4, b_xs], writes=[b_Y])
                        return Y, b_Y

                    def stage_b2(tc, yy):
                        Y, b_Y = yy
                        csl = slice(tc * 512, (tc + 1) * 512)
                        if g8 == 0:
                            p.op("act", lambda e: e.activation(out=yacc[:, csl], in_=Y[:], func=AF.Copy), reads=[b_Y], writes=[b_yacc])
                        else:
                            p.op("dve", lambda e: e.tensor_tensor(out=yacc[:, csl], in0=yacc[:, csl], in1=Y[:], op=ALU.add), reads=[b_Y, b_yacc], writes=[b_yacc])

                    ms, zs, ys = {}, {}, {}
                    for tc in range(12):
                        if tc < 8:
                            ms[tc] = stage_a1(tc)
                        if 1 <= tc < 9:
                            zs[tc - 1] = stage_a2(tc - 1, ms.pop(tc - 1))
                        if 2 <= tc < 10:
                            ys[tc - 2] = stage_b1(tc - 2, zs.pop(tc - 2))
                        if 3 <= tc < 11:
                            stage_b2(tc - 3, ys.pop(tc - 3))
                p.op("dve", lambda e: e.scalar_tensor_tensor(out=yacc[:], in0=uTt[:], scalar=dT[:, ct:ct + 1], in1=yacc[:], op0=ALU.mult, op1=ALU.add), reads=[b_uTt, b_dT, b_yacc], writes=[b_yacc])
                p.dma("sp", lambda e, ct=ct: e.dma_start(out=S["ysT"][ct * 128:(ct + 1) * 128, :], in_=yacc[:]), reads=[b_yacc], writes=[self.SB["ysT"]])

    def mix_glu(self, l):
        p, I, S = self.p, self.I, self.S
        with ExitStack() as ph:
            T = lambda name, shape, dt: self.T(ph, name, shape, dt)
            wgl, b_wgl = T("wgl", [128, 4, 512], BF16)
            wn = "w_glu_b%d" % l
            p.dma("sp", lambda e: e.dma_start(out=wgl[:], in_=S[wn].rearrange("(ct p) n -> p ct n", p=128)), reads=[self.SB[wn]], writes=[b_wgl])
            nsT, b_nsT = T("nsT", [128, 4], F32)
            p.dma("sp", lambda e: e.dma_start(out=nsT[:], in_=I["norm_ssm"][l].rearrange("(ct p) -> p ct", p=128), allow_slow_non_contiguous=True), writes=[b_nsT])
            epsr, b_epsr = T("epsr2", [128, 1], F32)
            p.op("pool", lambda e: e.memset(epsr[:], RMS_EPS), writes=[b_epsr])
            yrot = Rot(self, ph, "y4", [128, 4, 512], F32, 2)
            x2rot = Rot(self, ph, "gx2", [128, 4, 512], F32, 2)
            ygbrot = Rot(self, ph, "ygb", [128, 4, 512], BF16, 2)
            sgrot = Rot(self, ph, "sg", [128, 512], F32, 2)
            sqrot = Rot(self, ph, "sq", [128, 512], F32, 2)
            rsrot = Rot(self, ph, "rs5", [128, 512], F32, 2)
            onrot = Rot(self, ph, "onb", [128, 4, 512], BF16, 2)
            def glu_a(tc):
                self.emit_casts(4)
                csl = slice(tc * 512, (tc + 1) * 512)
                y4, b_y4 = yrot.next()
                x2, b_x2 = x2rot.next()
                ygb, b_ygb = ygbrot.next()
                p.dma("sp", lambda e: e.dma_start(out=y4[:], in_=S["ysT"][:, csl].rearrange("(ct p) t -> p ct t", p=128)), reads=[self.SB["ysT"]], writes=[b_y4])
                p.op("pool", lambda e: e.tensor_tensor(out=x2[:], in0=y4[:], in1=y4[:], op=ALU.mult), reads=[b_y4], writes=[b_x2])
                p.op("dve", lambda e: e.tensor_scalar(out=x2[:], in0=x2[:], scalar1=0.044715, scalar2=1.0, op0=ALU.mult, op1=ALU.add), reads=[b_x2], writes=[b_x2])
                p.op("pool", lambda e: e.tensor_tensor(out=x2[:], in0=x2[:], in1=y4[:], op=ALU.mult), reads=[b_x2, b_y4], writes=[b_x2])
                p.op("act", lambda e: e.activation(out=x2[:], in_=x2[:], func=AF.Exp, scale=-1.5957691216), reads=[b_x2], writes=[b_x2])
                p.op("dve", lambda e: e.tensor_scalar_add(out=x2[:], in0=x2[:], scalar1=1.0), reads=[b_x2], writes=[b_x2])
                p.op("dve", lambda e: e.reciprocal(out=x2[:], in_=x2[:]), reads=[b_x2], writes=[b_x2])
                p.op("dve", lambda e: e.tensor_tensor(out=y4[:], in0=y4[:], in1=x2[:], op=ALU.mult), reads=[b_x2, b_y4], writes=[b_y4])
                p.op("act", lambda e: e.activation(out=ygb[:], in_=y4[:], func=AF.Copy), reads=[b_y4], writes=[b_ygb])
                return y4, b_y4, ygb, b_ygb

            def glu_b(tc, c):
                y4, b_y4, ygb, b_ygb = c
                csl = slice(tc * 512, (tc + 1) * 512)
                pss, b_pss = self.pb(6 + tc % 2)
                for c2 in range(4):
                    pz, b_pz = self.pb(c2)
                    for ct in range(4):
                        p.op("pe", lambda e, ct=ct: e.matmul(pz[:], lhsT=wgl[:, ct, c2 * 128:(c2 + 1) * 128], rhs=ygb[:, ct, :], start=(ct == 0), stop=(ct == 3)), reads=[b_wgl, b_ygb], writes=[b_pz])
                    sg, b_sg = sgrot.next()
                    p.op("act", lambda e: e.activation(out=sg[:], in_=pz[:], func=AF.Exp, scale=-1.0), reads=[b_pz], writes=[b_sg])
                    p.op("dve", lambda e: e.tensor_scalar_add(out=sg[:], in0=sg[:], scalar1=1.0), reads=[b_sg], writes=[b_sg])
                    p.op("dve", lambda e: e.reciprocal(out=sg[:], in_=sg[:]), reads=[b_sg], writes=[b_sg])
                    p.op("dve", lambda e: e.tensor_tensor(out=y4[:, c2, :], in0=y4[:, c2, :], in1=sg[:], op=ALU.mult), reads=[b_sg, b_y4], writes=[b_y4])
                    sq, b_sq = sqrot.next()
                    p.op("pool", lambda e: e.tensor_tensor(out=sq[:], in0=y4[:, c2, :], in1=y4[:, c2, :], op=ALU.mult), reads=[b_y4], writes=[b_sq])
                    p.op("pe", lambda e: e.matmul(pss[:], lhsT=self.ones[:], rhs=sq[:], start=(c2 == 0), stop=(c2 == 3)), reads=[self.b_ones, b_sq], writes=[b_pss])
                rs, b_rs = rsrot.next()
                p.op("act", lambda e: e.activation(out=rs[:], in_=pss[:], func=AF.Ln, scale=1.0 / 512, bias=epsr[:]), reads=[b_pss, b_epsr], writes=[b_rs])
                p.op("act", lambda e: e.activation(out=rs[:], in_=rs[:], func=AF.Exp, scale=-0.5), reads=[b_rs], writes=[b_rs])
                onb, b_onb = onrot.next()
                for c2 in range(4):
                    p.op("dve", lambda e: e.scalar_tensor_tensor(out=onb[:, c2, :], in0=y4[:, c2, :], scalar=nsT[:, c2:c2 + 1], in1=rs[:], op0=ALU.mult, op1=ALU.mult), reads=[b_y4, b_nsT, b_rs], writes=[b_onb])
                p.dma("sp", lambda e: e.dma_start(out=S["mixT"][512:1024, csl].rearrange("(ct p) t -> p ct t", p=128), in_=onb[:]), reads=[b_onb], writes=[self.SB["mixT"]])

            ga = {0: glu_a(0)}
            for tc in range(8):
                if tc + 1 < 8:
                    ga[tc + 1] = glu_a(tc + 1)
                glu_b(tc, ga.pop(tc))

    def mix_out(self, l, src, b_src):
        p, I, S = self.p, self.I, self.S
        with ExitStack() as ph:
            T = lambda name, shape, dt: self.T(ph, name, shape, dt)
            wo, b_wo = T("wo", [128, 8, 1024], BF16)
            wn = "w_out_b%d" % l
            p.dma("sp", lambda e: e.dma_start(out=wo[:], in_=S[wn].rearrange("(kt p) n -> p kt n", p=128)), reads=[self.SB[wn]], writes=[b_wo])
            gB, b_gB = T("gB1", [128, 1024], F32)
            lng, b_lng = T("lng1", [128, 1024], F32)
            lnb, b_lnb = T("lnb1", [128, 1024], F32)
            self.load_bcast("sp", gB[:], b_gB, S["modrow"][l:l + 1, 2 * 1024:3 * 1024], reads=[self.SB["modrow"]])
            self.load_bcast("sp", lng[:], b_lng, I["ln_g"][l, 0:1, :])
            self.load_bcast("sp", lnb[:], b_lnb, I["ln_b"][l, 0:1, :])
            tmp = self.ln_tmps(ph)
            mrot = Rot(self, ph, "mixc", [128, 8, 512], BF16, 2)
            xrot = Rot(self, ph, "xin", [128, 1024], F32, 4)
            zrot = Rot(self, ph, "zt", [128, 1024], F32, 4)
            bk = [0]
            mix = {}

            def o_pre(TT):
                self.emit_casts(1)
                tc, tt = TT // 4, TT % 4
                t0 = TT * 128
                if tt == 0:
                    csl = slice(tc * 512, (tc + 1) * 512)
                    mix[tc] = mrot.next()
                    p.dma("sp", lambda e: e.dma_start(out=mix[tc][0][:], in_=S["mixT"][:, csl].rearrange("(kt p) t -> p kt t", p=128)), reads=[self.SB["mixT"]], writes=[mix[tc][1]])
                mixc, b_mixc = mix[tc]
                xt, b_xt = xrot.next()
                zt, b_zt = zrot.next()
                p.dma("sp", lambda e: e.dma_start(out=xt[:], in_=src[t0:t0 + 128, :]), reads=[b_src[TT]], writes=[b_xt])
                for hf in range(2):
                    py, b_py = self.pb(bk[0] % 4)
                    bk[0] += 1
                    for kt in range(8):
                        p.op("pe", lambda e, kt=kt: e.matmul(py[:], lhsT=mixc[:, kt, tt * 128:(tt + 1) * 128], rhs=wo[:, kt, hf * 512:(hf + 1) * 512], start=(kt == 0), stop=(kt == 7)), reads=[b_mixc, b_wo], writes=[b_py])
                    p.op("dve", lambda e: e.tensor_tensor(out=zt[:, hf * 512:(hf + 1) * 512], in0=py[:], in1=gB[:, hf * 512:(hf + 1) * 512], op=ALU.mult), reads=[b_py, b_gB], writes=[b_zt])
                if "mixdbg" in self.dbg:
                    p.dma("sp", lambda e: e.dma_start(out=self.S["mixdbg"][t0:t0 + 128, :], in_=zt[:]), reads=[b_zt], writes=[self.SB["mixdbg"]])
                p.op("dve", lambda e: e.scalar_tensor_tensor(out=zt[:], in0=xt[:], scalar=ALPHA, in1=zt[:], op0=ALU.mult, op1=ALU.add), reads=[b_xt, b_zt], writes=[b_zt])
                return xt, b_xt, zt, b_zt, self.ln_pre(zt, b_zt, tmp)

            def o_post(TT, c):
                t0 = TT * 128
                xt, b_xt, zt, b_zt, slot = c
                self.ln_post(zt, b_zt, slot, lng, b_lng, lnb, b_lnb, xt, b_xt)
                p.dma("sp", lambda e: e.dma_start(out=S["xs"][t0:t0 + 128, :], in_=xt[:]), reads=[b_xt], writes=[self.b_xs[TT]])

            cs_ = {}
            for TT in range(NT + 2):
                if TT < NT:
                    cs_[TT] = o_pre(TT)
                if TT >= 2:
                    o_post(TT - 2, cs_.pop(TT - 2))


_NC_CACHE = {}


def kernel(**inputs):
    if "nc" not in _NC_CACHE:
        _NC_CACHE["nc"] = MK().build()
    nc = _NC_CACHE["nc"]
    in_maps = []
    for b in range(8):
        m = {}
        for k in WSPEC:
            a = np.asarray(inputs[k], dtype=np.float32)
            if k in ("x", "c"):
                a = a[b]
            m[k] = np.ascontiguousarray(a)
        in_maps.append(m)
    res = run_bass_kernel_spmd(nc, in_maps, core_ids=list(range(8)))
    return np.stack([np.asarray(r["out"]) for r in res.results], axis=0).astype(np.float32)
```

```python
import numpy as np
import concourse.bass as bass
import concourse.mybir as mybir
from concourse.bass_utils import run_bass_kernel_spmd
from contextlib import ExitStack

F32 = mybir.dt.float32
BF16 = mybir.dt.bfloat16
I32 = mybir.dt.int32
AF = mybir.ActivationFunctionType
ALU = mybir.AluOpType
AX = mybir.AxisListType

EP = 30000
NDSEM = 40
NCSEM = 48

L = 4096
D = 1024
NT = 32
DFF = 2816
NFT = 22
DEPTH = 4
ALPHA = (2.0 * DEPTH) ** 0.25
LN_EPS = 1e-5
RMS_EPS = 1e-6
PROJ = 1816
C_Q, C_KC, C_VC, C_KS, C_VS, C_KW, C_VW, C_G, C_U = 0, 512, 640, 768, 896, 1024, 1152, 1280, 1304


class Buf:
    __slots__ = ("name", "w", "r", "excl")

    def __init__(self, name="", excl=False):
        self.name = name
        self.excl = excl
        self.w = None
        self.r = []


class Prog:
    ENG = ["pe", "act", "dve", "pool", "sp"]

    def __init__(self, nc, st):
        self.nc = nc
        self.st = st
        self.seq = {e: 0 for e in self.ENG}
        self.known = {e: {} for e in self.ENG}
        self.snaps = {e: [None] for e in self.ENG}
        self.esems = {e: [] for e in self.ENG}
        self.dsems = [st.enter_context(nc.semaphore("dq%d" % i)) for i in range(NDSEM)]
        self.dcount = 0
        self.csems = [st.enter_context(nc.semaphore("cq%d" % i)) for i in range(NCSEM)]
        self.ccount = 0
        self.dsnap = {}
        self.nwaits = 0
        self.engobj = {"pe": nc.tensor, "act": nc.scalar, "dve": nc.vector, "pool": nc.gpsimd, "sp": nc.sync}

    def _dsem(self, si):
        return self.dsems[si] if si < NDSEM else self.csems[si - NDSEM]

    def _esem(self, e, epoch):
        lst = self.esems[e]
        while len(lst) <= epoch:
            lst.append(self.st.enter_context(self.nc.semaphore("s_%s%d" % (e, len(lst)))))
        return lst[epoch]

    def _need(self, eng, ev, waits):
        if ev is None:
            return
        kn = self.known[eng]
        if ev[0] == "e":
            _, e2, n = ev
            if e2 == eng and eng == "pe":
                return
            key = ("e", e2)
            if kn.get(key, 0) >= n:
                return
            waits.append(ev)
            snap = self.snaps[e2][n]
            for k, v in snap.items():
                if kn.get(k, 0) < v:
                    kn[k] = v
            kn[key] = n
        else:
            _, si, val = ev
            key = ("d", si)
            if kn.get(key, 0) >= val:
                return
            waits.append(ev)
            kn[key] = val
            snap = self.dsnap.get((si, val))
            if snap:
                for k, v in snap.items():
                    if kn.get(k, 0) < v:
                        kn[k] = v

    def _deps(self, eng, reads, writes):
        waits = []
        for b in reads:
            self._need(eng, b.w, waits)
            if b.excl:
                for ev in b.r:
                    if not (ev[0] == "e" and ev[1] == eng):
                        self._need(eng, ev, waits)
        for b in writes:
            self._need(eng, b.w, waits)
            for ev in b.r:
                self._need(eng, ev, waits)
        return waits

    def op(self, eng, fn, reads=(), writes=()):
        waits = self._deps(eng, reads, writes)
        self.seq[eng] += 1
        n = self.seq[eng]
        ev = ("e", eng, n)
        self.snaps[eng].append(dict(self.known[eng]))
        for b in reads:
            b.r.append(ev)
        for b in writes:
            b.w = ev
            b.r = []
        self._emit(eng, waits, fn, ("e", n))
        return ev

    def dma(self, eng, fn, reads=(), writes=(), ring=0):
        if ring == 0:
            k = self.dcount
            self.dcount += 1
            si = k % NDSEM
            val = 16 * (k // NDSEM + 1)
            full = k >= NDSEM
        else:
            k = self.ccount
            self.ccount += 1
            si = NDSEM + k % NCSEM
            val = 16 * (k // NCSEM + 1)
            full = k >= NCSEM
        waits = self._deps(eng, reads, writes)
        if full:
            self._need(eng, ("d", si, val - 16), waits)
        ev = ("d", si, val)
        self.dsnap[(si, val)] = dict(self.known[eng])
        for b in reads:
            b.r.append(ev)
        for b in writes:
            b.w = ev
            b.r = []
        self._emit(eng, waits, fn, ("d", si))
        return ev

    def wait_all(self, eng, bufs):
        waits = []
        for b in bufs:
            self._need(eng, b.w, waits)
        self._emit(eng, waits, None, None)

    def barrier(self):
        for e in self.ENG:
            waits = []
            for e2 in self.ENG:
                if self.seq[e2]:
                    self._need(e, ("e", e2, self.seq[e2]), waits)
            for k in range(max(0, self.dcount - NDSEM), self.dcount):
                self._need(e, ("d", k % NDSEM, 16 * (k // NDSEM + 1)), waits)
            self._emit(e, waits, None, None)

    def _emit(self, e, waits, fn, kind):
        engine = self.engobj[e]
        for ev in waits:
            self.nwaits += 1
            if ev[0] == "e":
                _, e2, n = ev
                engine.wait_ge(self._esem(e2, (n - 1) // EP), (n - 1) % EP + 1)
            else:
                engine.wait_ge(self._dsem(ev[1]), ev[2])
        if fn is None:
            return
        inst = fn(engine)
        if kind[0] == "e":
            n = kind[1]
            inst.then_inc(self._esem(e, (n - 1) // EP), 1)
        else:
            inst.then_inc(self._dsem(kind[1]), 16)


class Rot:
    def __init__(self, mk, scope, name, shape, dt, n):
        self.items = [mk.T(scope, "%s%d" % (name, i), shape, dt) for i in range(n)]
        self.i = 0

    def next(self):
        it = self.items[self.i % len(self.items)]
        self.i += 1
        return it


WSPEC = {
    "x": [L, D], "c": [D], "w_in": [4, D, PROJ],
    "cmp_pos_k": [4, 32, 64], "cmp_pos_v": [4, 32, 64], "cmp_w1_k": [4, 32, 64, 64], "cmp_w2_k": [4, 64, 64],
    "cmp_w1_v": [4, 32, 64, 64], "cmp_w2_v": [4, 64, 64],
    "ssm_a_re": [4, 32, 64], "ssm_a_im": [4, 32, 64], "ssm_log_dt": [4, 32],
    "ssm_b_re": [4, 32, 64, 16], "ssm_b_im": [4, 32, 64, 16], "ssm_c_re": [4, 32, 16, 64], "ssm_c_im": [4, 32, 16, 64],
    "ssm_d": [4, 32, 16], "ssm_w_glu": [4, 512, 512], "norm_attn": [4, 512], "norm_ssm": [4, 512],
    "w_out": [4, D, D], "ada_w": [4, D, 6 * D], "ada_b": [4, 6 * D], "ln_g": [4, 2, D], "ln_b": [4, 2, D],
    "ffn_w_gate": [2, D, DFF], "ffn_w_up": [2, D, DFF], "ffn_w_down": [2, DFF, D],
    "moe_router": [2, D, 8], "moe_w_gate": [2, 8, D, DFF], "moe_w_up": [2, 8, D, DFF], "moe_w_down": [2, 8, DFF, D],
}


class MK:
    def __init__(self, dbg=None, phases=None):
        self.dbg = dbg or {}
        self.phases = phases
        self.nc = bass.Bass("TRN2", target_bir_lowering=False)
        self.I = {}
        nc = self.nc
        for k, shp in WSPEC.items():
            self.I[k] = nc.dram_tensor(k, shp, F32, kind="ExternalInput").ap()
        self.out = nc.dram_tensor("out", [L, D], F32, kind="ExternalOutput").ap()
        self.b_out = [Buf("out%d" % i) for i in range(NT)]
        self.b_xs = [Buf("xs%d" % i) for i in range(NT)]
        self.S = {}
        self.SB = {}

    def scratch(self, name, shape, dt):
        kind = "ExternalOutput" if name in self.dbg else "Internal"
        self.S[name] = self.nc.dram_tensor(name, shape, dt, kind=kind).ap()
        self.SB[name] = Buf(name)
        return self.S[name]

    def T(self, scope, name, shape, dt):
        self.tcount = getattr(self, "tcount", 0) + 1
        t = scope.enter_context(self.nc.sbuf_tensor("sb%d_%s" % (self.tcount, name), shape, dt))
        return t, Buf(name)

    def build(self):
        nc = self.nc
        with ExitStack() as st:
            self.p = p = Prog(nc, st)
            self.st = st
            self.ps = []
            for i in range(8):
                t = st.enter_context(nc.psum_tensor("ps%d" % i, [128, 512], F32))
                self.ps.append((t, Buf("ps%d" % i, excl=True)))
            self.psi = 0
            self.consts()
            self.scratch("xs", [L, D], F32)
            self.scratch("modrow", [4, 6 * D], F32)
            self.scratch("mixT", [D, L], BF16)
            self.scratch("uT", [512, L], BF16)
            self.scratch("ysT", [512, L], F32)
            for nm, shp, dt in (("kcmpT", [64, 2, 256], BF16), ("RCv", [128, 2, 2, 66], BF16), ("gsig", [128, NT, 24], F32),
                                ("seldbg", [NT, 2, 128, 64], F32), ("oattn", [L, 512], F32), ("mixdbg", [L, D], F32)):
                if nm in self.dbg:
                    self.scratch(nm, shp, dt)
            layers = list(self.dbg.get("layers", range(DEPTH)))
            self.cast_weights(layers[:1])
            self.emit_casts()
            self.ada_phase()
            p.barrier()
            for l in layers:
                src = self.I["x"] if l == layers[0] else self.S["xs"]
                srcb = [Buf("xin%d" % i) for i in range(NT)] if l == layers[0] else self.b_xs
                if l != layers[-1] and not (self.dbg.get("layers") is None and l >= 1):
                    self.cast_weights([layers[layers.index(l) + 1]])
                if self.phases is None or "mix" in self.phases:
                    self.mixer_phase(l, src, srcb)
                    p.barrier()
                    src, srcb = self.S["xs"], self.b_xs
                if self.phases is None or "ffn" in self.phases:
                    last = (l == layers[-1])
                    self.emit_casts()
                    self.ffn_phase(l, src, srcb, self.out if last else self.S["xs"], self.b_out if last else self.b_xs)
                    p.barrier()

            p.wait_all("sp", self.b_out + self.b_xs + [self.SB[k] for k in self.SB])
        return nc

    def bank(self):
        it = self.ps[self.psi % 8]
        self.psi += 1
        return it

    def consts(self):
        p, st = self.p, self.st
        self.reg0 = self.nc.gpsimd.to_reg(0.0)
        self.regp1 = self.nc.gpsimd.to_reg(1.0)
        self.regm1 = self.nc.gpsimd.to_reg(-1.0)
        self.ident, self.b_ident = self.T(st, "ident", [128, 128], F32)
        ident = self.ident
        p.op("pool", lambda e: e.memset(ident[:], 0.0), writes=[self.b_ident])
        p.op("pool", lambda e: e.affine_select(out=ident[:], in_=ident[:], pattern=[[-1, 128]], compare_op=ALU.not_equal,
                                               fill=self.regp1, base=0, channel_multiplier=1), reads=[self.b_ident], writes=[self.b_ident])
        self.ones, self.b_ones = self.T(st, "ones", [128, 128], F32)
        ones = self.ones
        p.op("pool", lambda e: e.memset(ones[:], 1.0), writes=[self.b_ones])
        self.modT, self.b_modT = self.T(st, "modT", [128, 4, 4, 8], F32)

    def emit_casts(self, n=None):
        k = len(self.pending) if n is None else min(n, len(self.pending))
        for fn in self.pending[:k]:
            fn()
        del self.pending[:k]

    def cast_weights(self, layers):
        p, I = self.p, self.I
        if not hasattr(self, "pending"):
            self.pending = []

        def cast(name, out_ap, in_ap):
            b = self.SB[name]
            self.pending.append(lambda: p.dma("pool", lambda e: e.dma_start(out=out_ap, in_=in_ap), writes=[b], ring=1))

        for l in layers:
            if self.phases is None or "mix" in self.phases:
                a = self.scratch("w_in_b%d" % l, [D, PROJ], BF16)
                cast("w_in_b%d" % l, a, I["w_in"][l])
                a = self.scratch("w_out_b%d" % l, [D, D], BF16)
                cast("w_out_b%d" % l, a, I["w_out"][l])
                a = self.scratch("w_glu_b%d" % l, [512, 512], BF16)
                cast("w_glu_b%d" % l, a, I["ssm_w_glu"][l])
            if not (self.phases is None or "ffn" in self.phases):
                continue
            li = l // 2
            srcs = [(I["ffn_w_gate"][li], I["ffn_w_up"][li], I["ffn_w_down"][li])] if l % 2 == 0 else \
                [(I["moe_w_gate"][li, e], I["moe_w_up"][li, e], I["moe_w_down"][li, e]) for e in range(8)]
            for e, (wg, wu, wd) in enumerate(srcs):
                for nm, w in (("g", wg), ("u", wu)):
                    name = "w%s_b%d_%d" % (nm, l, e)
                    a = self.scratch(name, [11, D, 256], BF16)
                    for fc in range(11):
                        cast(name, a[fc], w[:, fc * 256:(fc + 1) * 256])
                name = "wd_b%d_%d" % (l, e)
                a = self.scratch(name, [DFF, D], BF16)
                for h in range(2):
                    cast(name, a[h * 1408:(h + 1) * 1408, :], wd[h * 1408:(h + 1) * 1408, :])

    def ada_phase(self):
        p, I, nc = self.p, self.I, self.nc
        with ExitStack() as ph:
            cT, b_cT = self.T(ph, "cT", [128, 8], F32)
            ce, b_ce = self.T(ph, "ce", [128, 8], F32)
            p.dma("sp", lambda e: e.dma_start(out=cT[:], in_=I["c"].rearrange("(k p) -> p k", p=128), allow_slow_non_contiguous=True), writes=[b_cT])
            p.op("act", lambda e: e.activation(out=ce[:], in_=cT[:], func=AF.Exp, scale=-1.0), reads=[b_cT], writes=[b_ce])
            p.op("dve", lambda e: e.tensor_scalar_add(out=ce[:], in0=ce[:], scalar1=1.0), reads=[b_ce], writes=[b_ce])
            p.op("dve", lambda e: e.reciprocal(out=ce[:], in_=ce[:]), reads=[b_ce], writes=[b_ce])
            p.op("dve", lambda e: e.tensor_tensor(out=cT[:], in0=cT[:], in1=ce[:], op=ALU.mult), reads=[b_ce, b_cT], writes=[b_cT])
            wrot = Rot(self, ph, "adaw", [128, 8, 1024], F32, 2)
            brot = Rot(self, ph, "adab", [1, 1024], F32, 2)
            rrot = Rot(self, ph, "adar", [1, 1024], F32, 2)
            for l in self.dbg.get("layers", range(DEPTH)):
                for s in range(6):
                    wt, b_wt = wrot.next()
                    bt, b_bt = brot.next()
                    rt, b_rt = rrot.next()
                    p.dma("sp", lambda e, wt=wt, l=l, s=s: e.dma_start(out=wt[:], in_=I["ada_w"][l][:, s * 1024:(s + 1) * 1024].rearrange("(kt p) n -> p kt n", p=128)), writes=[b_wt])
                    p.dma("sp", lambda e, bt=bt, l=l, s=s: e.dma_start(out=bt[:], in_=I["ada_b"][l:l + 1, s * 1024:(s + 1) * 1024]), writes=[b_bt])
                    for hf in range(2):
                        pt, b_pt = self.bank()
                        for kt in range(8):
                            p.op("pe", lambda e, pt=pt, kt=kt, wt=wt, hf=hf: e.matmul(pt[0:1, :], lhsT=cT[:, kt:kt + 1], rhs=wt[:, kt, hf * 512:(hf + 1) * 512], start=(kt == 0), stop=(kt == 7)),
                                 reads=[b_cT, b_wt], writes=[b_pt])
                        addc = 1.0 if s in (1, 2, 4, 5) else 0.0
                        p.op("dve", lambda e, pt=pt, bt=bt, rt=rt, hf=hf, addc=addc: e.scalar_tensor_tensor(out=rt[:, hf * 512:(hf + 1) * 512], in0=pt[0:1, :], scalar=addc, in1=bt[:, hf * 512:(hf + 1) * 512], op0=ALU.add, op1=ALU.add),
                             reads=[b_pt, b_bt], writes=[b_rt])
                    p.dma("sp", lambda e, rt=rt, l=l, s=s: e.dma_start(out=self.S["modrow"][l:l + 1, s * 1024:(s + 1) * 1024], in_=rt[:]), reads=[b_rt], writes=[self.SB["modrow"]])
                    if s in (0, 1, 3, 4):
                        si = {0: 0, 1: 1, 3: 2, 4: 3}[s]
                        pt, b_pt = self.bank()
                        for j in range(8):
                            p.op("pe", lambda e, pt=pt, rt=rt, j=j: e.matmul(pt[:, j:j + 1], lhsT=rt[0:1, j * 128:(j + 1) * 128], rhs=self.ones[0:1, 0:1], start=True, stop=True),
                                 reads=[b_rt, self.b_ones], writes=[b_pt])
                        p.op("dve", lambda e, pt=pt, l=l, si=si: e.tensor_copy(out=self.modT[:, l, si, :], in_=pt[:, 0:8]), reads=[b_pt], writes=[self.b_modT])

    def load_bcast(self, eng, dst, b_dst, src_row, reads=()):
        self.p.dma(eng, lambda e: e.dma_start(out=dst, in_=src_row.partition_broadcast(128)), reads=list(reads), writes=[b_dst])

    def ln_tmps(self, ph, n=4):
        self.eps_ln, self.b_eps = self.T(ph, "epsln", [128, 1], F32)
        self.p.op("pool", lambda e: e.memset(self.eps_ln[:], LN_EPS), writes=[self.b_eps])
        slots = []
        for i in range(n):
            st6, b_st6 = self.T(ph, "st6_%d" % i, [128, 2, 6], F32)
            mv, b_mv = self.T(ph, "mv_%d" % i, [128, 2], F32)
            rs, b_rs = self.T(ph, "rs_%d" % i, [128, 1], F32)
            slots.append((st6, b_st6, mv, b_mv, rs, b_rs))
        self.ln_i = 0
        return slots

    def ln_pre(self, z, b_z, slots):
        p = self.p
        slot = slots[self.ln_i % len(slots)]
        self.ln_i += 1
        st6, b_st6, mv, b_mv, rs, b_rs = slot
        for hf in range(2):
            p.op("dve", lambda e, hf=hf: e.bn_stats(out=st6[:, hf, :], in_=z[:, hf * 512:(hf + 1) * 512]), reads=[b_z], writes=[b_st6])
        p.op("dve", lambda e: e.bn_aggr(out=mv[:], in_=st6[:]), reads=[b_st6], writes=[b_mv])
        p.op("act", lambda e: e.activation(out=rs[:], in_=mv[:, 1:2], func=AF.Ln, bias=self.eps_ln[:], scale=1.0), reads=[b_mv, self.b_eps], writes=[b_rs])
        p.op("act", lambda e: e.activation(out=rs[:], in_=rs[:], func=AF.Exp, scale=-0.5), reads=[b_rs], writes=[b_rs])
        return slot

    def ln_post(self, z, b_z, slot, lng, b_lng, lnb, b_lnb, outt, b_out):
        p = self.p
        st6, b_st6, mv, b_mv, rs, b_rs = slot
        p.op("dve", lambda e: e.scalar_tensor_tensor(out=z[:], in0=z[:], scalar=mv[:, 0:1], in1=lng[:], op0=ALU.subtract, op1=ALU.mult), reads=[b_z, b_mv, b_lng], writes=[b_z])
        p.op("dve", lambda e: e.scalar_tensor_tensor(out=outt[:], in0=z[:], scalar=rs[:, 0:1], in1=lnb[:], op0=ALU.mult, op1=ALU.add), reads=[b_z, b_rs, b_lnb], writes=[b_out])

    def transpose_mod(self, xt, b_xt, dst_fn, b_dst, l, si_sh, si_sc):
        p = self.p
        for hf in range(2):
            pt, b_pt = self.bank()
            for j in range(4):
                kt = hf * 4 + j
                p.op("pe", lambda e, pt=pt, j=j, kt=kt: e.transpose(out=pt[:, j * 128:(j + 1) * 128], in_=xt[:, kt * 128:(kt + 1) * 128], identity=self.ident[:]),
                     reads=[b_xt, self.b_ident], writes=[b_pt])
            for j in range(4):
                kt = hf * 4 + j
                p.op("act", lambda e, pt=pt, j=j, kt=kt: e.activation(out=dst_fn(kt), in_=pt[:, j * 128:(j + 1) * 128], func=AF.Identity,
                                                                   scale=self.modT[:, l, si_sc, kt:kt + 1], bias=self.modT[:, l, si_sh, kt:kt + 1]),
                     reads=[b_pt, self.b_modT], writes=[b_dst])

    def ffn_phase(self, l, src, b_src, dst, b_dst):
        p, I, S = self.p, self.I, self.S
        if l == 1 and self.dbg.get("layers") is None:
            self.cast_weights([2, 3])
        moe = (l % 2 == 1)
        li = l // 2
        E = 8 if moe else 1
        with ExitStack() as ph:
            hT, _ = self.T(ph, "hT", [128, 8, 1024], BF16)
            b_hT = [Buf("hT%d" % i) for i in range(8)]
            acc, _ = self.T(ph, "acc", [128, 8, 1024], F32)
            b_acc = [Buf("acc%d" % i) for i in range(8)]
            act, _ = self.T(ph, "actT", [128, NFT, 1024], BF16)
            b_act = [Buf("act%d" % i) for i in range(NFT)]
            wd, _ = self.T(ph, "wd", [128, NFT, 1024], BF16)
            b_wd = [Buf("wd0"), Buf("wd1")]
            wgrot = Rot(self, ph, "wg", [128, 8, 256], BF16, 2)
            wurot = Rot(self, ph, "wu", [128, 8, 256], BF16, 2)
            xrot = Rot(self, ph, "xin", [128, 1024], F32, 3)
            zrot = Rot(self, ph, "zt", [128, 1024], F32, 3)
            srot = Rot(self, ph, "silu", [128, 512], BF16, 3)
            gB, b_gB = self.T(ph, "gB", [128, 1024], F32)
            lng, b_lng = self.T(ph, "lng", [128, 1024], F32)
            lnb, b_lnb = self.T(ph, "lnb", [128, 1024], F32)
            tmp = self.ln_tmps(ph)
            self.load_bcast("sp", gB[:], b_gB, S["modrow"][l:l + 1, 5 * 1024:6 * 1024], reads=[self.SB["modrow"]])
            self.load_bcast("sp", lng[:], b_lng, I["ln_g"][l, 1:2, :])
            self.load_bcast("sp", lnb[:], b_lnb, I["ln_b"][l, 1:2, :])
            if moe:
                rt, b_rt = self.T(ph, "rt", [128, 8, 8], F32)
                p.dma("sp", lambda e: e.dma_start(out=rt[:], in_=I["moe_router"][li].rearrange("(kt p) e -> p kt e", p=128)), writes=[b_rt])
                gate, b_gate = self.T(ph, "gate", [128, 2, 8, 8], F32)
                h32rot = Rot(self, ph, "h32", [128, 8, 128], F32, 2)
                lg, b_lg = self.T(ph, "lg", [128, 8], F32)
                srt, b_srt = self.T(ph, "srt", [128, 8], F32)
                gw, b_gw = self.T(ph, "gw", [128, 4], F32)
                g2t, b_g2t = self.T(ph, "g2t", [128, 8], F32)
            def F0(tg):
                for tt in range(8):
                    t0 = tg * 1024 + tt * 128
                    xt, b_xt = xrot.next()
                    p.dma("sp", lambda e, xt=xt, t0=t0: e.dma_start(out=xt[:], in_=src[t0:t0 + 128, :]), reads=[b_src[tg * 8 + tt]], writes=[b_xt])
                    if not moe:
                        self.transpose_mod(xt, b_xt, lambda kt, tt=tt: hT[:, kt, tt * 128:(tt + 1) * 128], b_hT[tt], l, 2, 3)
                    else:
                        h32, b_h32 = h32rot.next()
                        self.transpose_mod(xt, b_xt, lambda kt, h32=h32: h32[:, kt, :], b_h32, l, 2, 3)
                        pt, b_pt = self.bank()
                        for kt in range(8):
                            p.op("pe", lambda e, pt=pt, kt=kt, h32=h32: e.matmul(pt[:, 0:8], lhsT=h32[:, kt, :], rhs=rt[:, kt, :], start=(kt == 0), stop=(kt == 7)),
                                 reads=[b_h32, b_rt], writes=[b_pt])
                        p.op("dve", lambda e, h32=h32, tt=tt: e.tensor_copy(out=hT[:, :, tt * 128:(tt + 1) * 128], in_=h32[:]), reads=[b_h32], writes=[b_hT[tt]])
                        p.op("dve", lambda e, pt=pt: e.tensor_copy(out=lg[:], in_=pt[:, 0:8]), reads=[b_pt], writes=[b_lg])
                        p.op("dve", lambda e: e.max(out=srt[:], in_=lg[:]), reads=[b_lg], writes=[b_srt])
                        p.op("dve", lambda e: e.tensor_tensor(out=gw[:, 0:1], in0=srt[:, 1:2], in1=srt[:, 0:1], op=ALU.subtract), reads=[b_srt], writes=[b_gw])
                        p.op("act", lambda e: e.activation(out=gw[:, 1:2], in_=gw[:, 0:1], func=AF.Exp), reads=[b_gw], writes=[b_gw])
                        p.op("dve", lambda e: e.tensor_scalar_add(out=gw[:, 2:3], in0=gw[:, 1:2], scalar1=1.0), reads=[b_gw], writes=[b_gw])
                        p.op("dve", lambda e: e.reciprocal(out=gw[:, 2:3], in_=gw[:, 2:3]), reads=[b_gw], writes=[b_gw])
                        p.op("dve", lambda e: e.tensor_tensor(out=gw[:, 3:4], in0=gw[:, 1:2], in1=gw[:, 2:3], op=ALU.mult), reads=[b_gw], writes=[b_gw])
                        p.op("dve", lambda e: e.tensor_scalar(out=g2t[:], in0=lg[:], scalar1=srt[:, 1:2], scalar2=gw[:, 3:4], op0=ALU.is_equal, op1=ALU.mult),
                             reads=[b_lg, b_srt, b_gw], writes=[b_g2t])
                        p.op("dve", lambda e, tt=tt: e.tensor_scalar(out=gate[:, tg % 2, tt, :], in0=lg[:], scalar1=srt[:, 0:1], scalar2=gw[:, 2:3], op0=ALU.is_equal, op1=ALU.mult),
                             reads=[b_lg, b_srt, b_gw], writes=[b_gate])
                        p.op("dve", lambda e, tt=tt: e.tensor_tensor(out=gate[:, tg % 2, tt, :], in0=gate[:, tg % 2, tt, :], in1=g2t[:], op=ALU.add), reads=[b_g2t, b_gate], writes=[b_gate])
            F0(0)
            for tg in range(4):
                def f2_pre(tt):
                    t0 = tg * 1024 + tt * 128
                    xt, b_xt = xrot.next()
                    zt, b_zt = zrot.next()
                    p.dma("sp", lambda e: e.dma_start(out=xt[:], in_=src[t0:t0 + 128, :]), reads=[b_src[tg * 8 + tt]], writes=[b_xt])
                    p.op("dve", lambda e: e.tensor_tensor(out=zt[:], in0=acc[:, tt, :], in1=gB[:], op=ALU.mult), reads=[b_acc[tt], b_gB], writes=[b_zt])
                    p.op("dve", lambda e: e.scalar_tensor_tensor(out=zt[:], in0=xt[:], scalar=ALPHA, in1=zt[:], op0=ALU.mult, op1=ALU.add), reads=[b_xt, b_zt], writes=[b_zt])
                    return xt, b_xt, zt, b_zt, self.ln_pre(zt, b_zt, tmp)

                def f2_post(tt, c):
                    t0 = tg * 1024 + tt * 128
                    xt, b_xt, zt, b_zt, slot = c
                    self.ln_post(zt, b_zt, slot, lng, b_lng, lnb, b_lnb, xt, b_xt)
                    p.dma("sp", lambda e: e.dma_start(out=dst[t0:t0 + 128, :], in_=xt[:]), reads=[b_xt], writes=[b_dst[tg * 8 + tt]])

                cs_ = {}
                for ex in range(E):
                    wdn = "wd_b%d_%d" % (l, ex)
                    for h in range(2):
                        p.dma("sp", lambda e, wdn=wdn, h=h: e.dma_start(out=wd[:, h * 11:(h + 1) * 11, :], in_=S[wdn][h * 1408:(h + 1) * 1408, :].rearrange("(ft p) n -> p ft n", p=128)),
                              reads=[self.SB[wdn]], writes=[b_wd[h]])
                    for fc in range(11):
                        self.emit_casts(1)
                        wg, b_wg = wgrot.next()
                        wu, b_wu = wurot.next()
                        gn, un = "wg_b%d_%d" % (l, ex), "wu_b%d_%d" % (l, ex)
                        p.dma("sp", lambda e, wg=wg, gn=gn, fc=fc: e.dma_start(out=wg[:], in_=S[gn][fc].rearrange("(kt p) f -> p kt f", p=128)), reads=[self.SB[gn]], writes=[b_wg])
                        p.dma("sp", lambda e, wu=wu, un=un, fc=fc: e.dma_start(out=wu[:], in_=S[un][fc].rearrange("(kt p) f -> p kt f", p=128)), reads=[self.SB[un]], writes=[b_wu])
                        for f2 in range(2):
                            ft = fc * 2 + f2
                            for tc in range(2):
                                pg, b_pg = self.bank()
                                pu, b_pu = self.bank()
                                hb = b_hT[tc * 4:(tc + 1) * 4]
                                for kt in range(8):
                                    p.op("pe", lambda e, pg=pg, wg=wg, kt=kt, f2=f2, tc=tc: e.matmul(pg[:], lhsT=wg[:, kt, f2 * 128:(f2 + 1) * 128], rhs=hT[:, kt, tc * 512:(tc + 1) * 512], start=(kt == 0), stop=(kt == 7)),
                                         reads=[b_wg] + hb, writes=[b_pg])
                                for kt in range(8):
                                    p.op("pe", lambda e, pu=pu, wu=wu, kt=kt, f2=f2, tc=tc: e.matmul(pu[:], lhsT=wu[:, kt, f2 * 128:(f2 + 1) * 128], rhs=hT[:, kt, tc * 512:(tc + 1) * 512], start=(kt == 0), stop=(kt == 7)),
                                         reads=[b_wu] + hb, writes=[b_pu])
                                sl, b_sl = srot.next()
                                p.op("act", lambda e, sl=sl, pg=pg: e.activation(out=sl[:], in_=pg[:], func=AF.Silu), reads=[b_pg], writes=[b_sl])
                                p.op("dve", lambda e, sl=sl, pu=pu, ft=ft, tc=tc: e.tensor_tensor(out=act[:, ft, tc * 512:(tc + 1) * 512], in0=sl[:], in1=pu[:], op=ALU.mult),
                                     reads=[b_sl, b_pu], writes=[b_act[ft]])
                    if ex == E - 1 and tg + 1 < 4:
                        F0(tg + 1)
                    for tt in range(8):
                        for hf in range(2):
                            pd, b_pd = self.bank()
                            for ft in range(NFT):
                                p.op("pe", lambda e, pd=pd, ft=ft, tt=tt, hf=hf: e.matmul(pd[:], lhsT=act[:, ft, tt * 128:(tt + 1) * 128], rhs=wd[:, ft, hf * 512:(hf + 1) * 512], start=(ft == 0), stop=(ft == NFT - 1)),
                                     reads=[b_act[ft], b_wd[ft // 11]], writes=[b_pd])
                            a_sl = acc[:, tt, hf * 512:(hf + 1) * 512]
                            if not moe:
                                p.op("dve", lambda e, pd=pd, a_sl=a_sl: e.tensor_copy(out=a_sl, in_=pd[:]), reads=[b_pd], writes=[b_acc[tt]])
                            elif ex == 0:
                                p.op("dve", lambda e, pd=pd, a_sl=a_sl, tt=tt, ex=ex: e.tensor_scalar(out=a_sl, in0=pd[:], scalar1=gate[:, tg % 2, tt, ex:ex + 1], scalar2=None, op0=ALU.mult),
                                     reads=[b_pd, b_gate], writes=[b_acc[tt]])
                            else:
                                p.op("dve", lambda e, pd=pd, a_sl=a_sl, tt=tt, ex=ex: e.scalar_tensor_tensor(out=a_sl, in0=pd[:], scalar=gate[:, tg % 2, tt, ex:ex + 1], in1=a_sl, op0=ALU.mult, op1=ALU.add),
                                     reads=[b_pd, b_gate, b_acc[tt]], writes=[b_acc[tt]])
                        if ex == E - 1:
                            cs_[tt] = f2_pre(tt)
                            if tt >= 2:
                                f2_post(tt - 2, cs_.pop(tt - 2))
                    if ex == E - 1:
                        f2_post(6, cs_.pop(6))
                        f2_post(7, cs_.pop(7))

    def pb(self, i):
        return self.ps[i]

    def evac(self, k, out_ap, in_ap, reads, writes):
        if k % 2 == 0:
            self.p.op("act", lambda e: e.activation(out=out_ap, in_=in_ap, func=AF.Copy), reads=reads, writes=writes)
        else:
            self.p.op("dve", lambda e: e.tensor_copy(out=out_ap, in_=in_ap), reads=reads, writes=writes)

    def mixer_phase(self, l, src, b_src):
        sub = self.dbg.get("mixsub", ("attn", "ssm", "glu", "out"))
        self.attn_cast_n = 2 if (self.dbg.get("layers") is None and self.phases is None) else 7
        if "attn" in sub:
            self.mix_attn(l, src, b_src)
            self.p.barrier()
        if "ssm" in sub:
            self.mix_ssm(l)
            self.p.barrier()
        if "glu" in sub:
            self.mix_glu(l)
            self.p.barrier()
        if "out" in sub:
            self.mix_out(l, src, b_src)

    def mix_attn(self, l, src, b_src):
        p, I, S = self.p, self.I, self.S
        with ExitStack() as ph:
            T = lambda name, shape, dt: self.T(ph, name, shape, dt)
            qT, _ = T("qT", [64, 8, L], BF16)
            b_qT = [Buf("qT%d" % i) for i in range(8)]
            KX, _ = T("KX", [128, 2, L], BF16)
            b_KX = [Buf("KX%d" % i) for i in range(8)]
            b_KXc = Buf("KXc")
            kwT, _ = T("kwT", [64, 2, L], BF16)
            b_kwT = [Buf("kwT%d" % i) for i in range(8)]
            kcT, b_kcT = T("kcT", [128, L], BF16)
            vcT, b_vcT = T("vcT", [128, L], BF16)
            vsA, _ = T("vsA", [128, NT, 2, 66], BF16)
            b_vsA = [Buf("vsA%d" % i) for i in range(NT)]
            vwA, _ = T("vwA", [128, NT, 2, 66], BF16)
            b_vwA = [Buf("vwA%d" % i) for i in range(NT)]
            b_one = Buf("vones")
            gsig, _ = T("gsig", [128, NT, 24], F32)
            b_gs = [Buf("gs%d" % i) for i in range(NT)]
            kcmpT, b_kcmpT = T("kcmpT", [64, 2, 256], BF16)
            RCv, b_RCv = T("RCv", [128, 2, 2, 66], BF16)
            ov, b_ov = T("ov", [128, 2, 64], BF16)
            p.op("pool", lambda e: e.memset(KX[64:128], 1.0), writes=[b_KXc])
            p.op("pool", lambda e: e.affine_select(out=KX[64:128], in_=KX[64:128], pattern=[[0, 2], [1, L]], compare_op=ALU.is_ge, fill=self.reg0, base=0, channel_multiplier=-64), reads=[b_KXc], writes=[b_KXc])
            p.op("pool", lambda e: e.affine_select(out=KX[64:128], in_=KX[64:128], pattern=[[0, 2], [-1, L]], compare_op=ALU.is_ge, fill=self.reg0, base=63, channel_multiplier=64), reads=[b_KXc], writes=[b_KXc])
            p.op("pool", lambda e: e.memset(vsA[:, :, :, 64:65], 1.0), writes=[b_one])
            p.op("pool", lambda e: e.memset(vwA[:, :, :, 64:65], 1.0), writes=[b_one])
            p.op("pool", lambda e: e.memset(RCv[:], 0.0), writes=[b_RCv])
            p.op("pool", lambda e: e.memset(RCv[:, :, :, 64:65], 1.0), reads=[b_RCv], writes=[b_RCv])
            p.op("pool", lambda e: e.memset(ov[:], 1.0), writes=[b_ov])
            for nt in range(2):
                p.op("pool", lambda e, nt=nt: e.affine_select(out=ov[:, nt, :], in_=ov[:, nt, :], pattern=[[-4, 64]], compare_op=ALU.is_ge, fill=self.reg0, base=1 + 128 * nt, channel_multiplier=1), reads=[b_ov], writes=[b_ov])
                p.op("pool", lambda e, nt=nt: e.affine_select(out=ov[:, nt, :], in_=ov[:, nt, :], pattern=[[4, 64]], compare_op=ALU.is_ge, fill=self.reg0, base=3 - 128 * nt, channel_multiplier=-1), reads=[b_ov], writes=[b_ov])
            with ExitStack() as p1:
                win, b_win = self.T(p1, "win", [128, 8, PROJ], BF16)
                wn = "w_in_b%d" % l
                p.dma("sp", lambda e: e.dma_start(out=win[:], in_=S[wn].rearrange("(kt p) n -> p kt n", p=128)), reads=[self.SB[wn]], writes=[b_win])
                hrot = Rot(self, p1, "hTc", [128, 8, 512], BF16, 2)
                xrot = Rot(self, p1, "xin", [128, 1024], F32, 2)
                urot = Rot(self, p1, "uTc", [128, 4, 512], BF16, 2)
                ek = 0
                for tc in range(8):
                    hTc, b_hTc = hrot.next()
                    csl = slice(tc * 512, (tc + 1) * 512)
                    for tt in range(4):
                        t0 = tc * 512 + tt * 128
                        xt, b_xt = xrot.next()
                        p.dma("sp", lambda e, xt=xt, t0=t0: e.dma_start(out=xt[:], in_=src[t0:t0 + 128, :]), reads=[b_src[tc * 4 + tt]], writes=[b_xt])
                        self.transpose_mod(xt, b_xt, lambda kt, tt=tt, hTc=hTc: hTc[:, kt, tt * 128:(tt + 1) * 128], b_hTc, l, 0, 1)
                    uTc, b_uTc = urot.next()
                    groups = []
                    for hd in range(8):
                        groups.append((C_Q + 64 * hd, 64, qT[:, hd, csl], b_qT[tc]))
                    for h in range(2):
                        groups.append((C_KS + 64 * h, 64, KX[0:64, h, csl], b_KX[tc]))
                        groups.append((C_KW + 64 * h, 64, kwT[:, h, csl], b_kwT[tc]))
                    groups.append((C_KC, 128, kcT[:, csl], b_kcT))
                    groups.append((C_VC, 128, vcT[:, csl], b_vcT))
                    for ct in range(4):
                        groups.append((C_U + 128 * ct, 128, uTc[:, ct, :], b_uTc))
                    if self.dbg.get("m1lvl", 9) < 1:
                        groups = []
                    if self.dbg.get("m1lvl", 9) == 1:
                        groups = groups[:8]
                    if self.dbg.get("m1lvl", 9) == 2:
                        groups = groups[:14]
                    for gi, (c0, wdt, dst, b_d) in enumerate(groups):
                        pt, b_pt = self.pb(gi % 4)
                        for kt in range(8):
                            p.op("pe", lambda e, pt=pt, kt=kt, c0=c0, wdt=wdt, hTc=hTc: e.matmul(pt[0:wdt, :], lhsT=win[:, kt, c0:c0 + wdt], rhs=hTc[:, kt, :], start=(kt == 0), stop=(kt == 7)),
                                 reads=[b_win, b_hTc], writes=[b_pt])
                        self.evac(ek, dst, pt[0:wdt, :], [b_pt], [b_d])
                        ek += 1
                    p.dma("sp", lambda e, uTc=uTc, csl=csl: e.dma_start(out=S["uT"][:, csl].rearrange("(ct p) t -> p ct t", p=128), in_=uTc[:]), reads=[b_uTc], writes=[self.SB["uT"]])
                    for tt in range(4 if self.dbg.get("m1lvl", 9) > 3 else 0):
                        TT = tc * 4 + tt
                        pt, b_pt = self.pb(4 + tt % 2)
                        for kt in range(8):
                            p.op("pe", lambda e, pt=pt, kt=kt, tt=tt, hTc=hTc: e.matmul(pt[:, 0:408], lhsT=hTc[:, kt, tt * 128:(tt + 1) * 128], rhs=win[:, kt, C_VS:C_VS + 408], start=(kt == 0), stop=(kt == 7)),
                                 reads=[b_win, b_hTc], writes=[b_pt])
                        if self.dbg.get("m1lvl", 9) >= 5:
                            p.op("act", lambda e, pt=pt, TT=TT: e.activation(out=vsA[:, TT, :, 0:64], in_=pt[:, 0:128].rearrange("p (h d) -> p h d", h=2), func=AF.Copy), reads=[b_pt], writes=[b_vsA[TT]])
                        if self.dbg.get("m1lvl", 9) >= 6:
                            p.op("dve", lambda e, pt=pt, TT=TT: e.tensor_copy(out=vwA[:, TT, :, 0:64], in_=pt[:, 256:384].rearrange("p (h d) -> p h d", h=2)), reads=[b_pt], writes=[b_vwA[TT]])
                        if self.dbg.get("m1lvl", 9) >= 7:
                            p.op("act", lambda e, pt=pt, TT=TT: e.activation(out=gsig[:, TT, :], in_=pt[:, 384:408], func=AF.Exp, scale=-1.0), reads=[b_pt], writes=[b_gs[TT]])
                        if self.dbg.get("m1lvl", 9) >= 8:
                            p.op("dve", lambda e, TT=TT: e.tensor_scalar_add(out=gsig[:, TT, :], in0=gsig[:, TT, :], scalar1=1.0), reads=[b_gs[TT]], writes=[b_gs[TT]])
                        if self.dbg.get("m1lvl", 9) >= 8:
                            p.op("dve", lambda e, TT=TT: e.reciprocal(out=gsig[:, TT, :], in_=gsig[:, TT, :]), reads=[b_gs[TT]], writes=[b_gs[TT]])
            p.barrier()
            if self.dbg.get("attn_stop") == "m1":
                return
            with ExitStack() as p1:
                w1s, b_w1s = self.T(p1, "w1s", [128, 32, 64], F32)
                w1b, b_w1b = self.T(p1, "w1b", [128, 32, 64], BF16)
                w2s, b_w2s = self.T(p1, "w2s", [64, 64], F32)
                w2b, b_w2b = self.T(p1, "w2b", [64, 64], BF16)
                pss, b_pss = self.T(p1, "pss", [64, 32], F32)
                psb, b_psb = self.T(p1, "psb", [64, 32], BF16)
                cbias, b_cbias = self.T(p1, "cbias", [64, 1], F32)
                xh, b_xh = self.T(p1, "xh", [64, 256], F32)
                x2, b_x2 = self.T(p1, "x2", [64, 256], F32)
                hg, b_hg = self.T(p1, "hg", [64, 256], BF16)
                p.op("pool", lambda e: e.memset(hg[:], 0.0), writes=[b_hg])
                for kv, (w1n, w2n, posn, srcT, b_srcT) in enumerate((("cmp_w1_k", "cmp_w2_k", "cmp_pos_k", kcT, b_kcT), ("cmp_w1_v", "cmp_w2_v", "cmp_pos_v", vcT, b_vcT))):
                    for hh in range(2):
                        p.dma("sp", lambda e, hh=hh, w1n=w1n: e.dma_start(out=w1s[hh * 64:(hh + 1) * 64], in_=I[w1n][l].rearrange("l d f -> d l f")), writes=[b_w1s])
                    p.dma("sp", lambda e, w2n=w2n: e.dma_start(out=w2s[:], in_=I[w2n][l]), writes=[b_w2s])
                    p.dma("sp", lambda e, posn=posn: e.dma_start(out=pss[:], in_=I[posn][l].rearrange("l d -> d l"), allow_slow_non_contiguous=True), writes=[b_pss])
                    p.op("dve", lambda e: e.tensor_copy(out=w1b[:], in_=w1s[:]), reads=[b_w1s], writes=[b_w1b])
                    p.op("dve", lambda e: e.tensor_copy(out=w2b[:], in_=w2s[:]), reads=[b_w2s], writes=[b_w2b])
                    p.op("dve", lambda e: e.tensor_copy(out=psb[:], in_=pss[:]), reads=[b_pss], writes=[b_psb])
                    pt, b_pt = self.pb(0)
                    for ll in range(32):
                        p.op("pe", lambda e, pt=pt, ll=ll: e.matmul(pt[0:64, 0:1], lhsT=w1b[0:64, ll, :], rhs=psb[:, ll:ll + 1], start=(ll == 0), stop=(ll == 31)), reads=[b_w1b, b_psb], writes=[b_pt])
                    p.op("dve", lambda e, pt=pt: e.tensor_copy(out=cbias[:], in_=pt[0:64, 0:1]), reads=[b_pt], writes=[b_cbias])
                    for h in range(2):
                        pt, b_pt = self.pb(1 + h)
                        for ll in range(32):
                            p.op("pe", lambda e, pt=pt, ll=ll, h=h, srcT=srcT: e.matmul(pt[0:64, 0:255], lhsT=w1b[64 * h:64 * h + 64, ll, :], rhs=srcT[64 * h:64 * h + 64, ll:ll + 16 * 254 + 1:16], start=(ll == 0), stop=(ll == 31)),
                                 reads=[b_w1b, b_srcT], writes=[b_pt])
                        xv, x2v = xh[:, 0:255], x2[:, 0:255]
                        p.op("dve", lambda e, pt=pt: e.tensor_scalar(out=xv, in0=pt[0:64, 0:255], scalar1=cbias[:, 0:1], scalar2=None, op0=ALU.add), reads=[b_pt, b_cbias], writes=[b_xh])
                        p.op("dve", lambda e: e.tensor_tensor(out=x2v, in0=xv, in1=xv, op=ALU.mult), reads=[b_xh], writes=[b_x2])
                        p.op("dve", lambda e: e.tensor_scalar(out=x2v, in0=x2v, scalar1=0.044715, scalar2=1.0, op0=ALU.mult, op1=ALU.add), reads=[b_x2], writes=[b_x2])
                        p.op("dve", lambda e: e.tensor_tensor(out=x2v, in0=x2v, in1=xv, op=ALU.mult), reads=[b_x2, b_xh], writes=[b_x2])
                        p.op("act", lambda e: e.activation(out=x2v, in_=x2v, func=AF.Exp, scale=-1.5957691216), reads=[b_x2], writes=[b_x2])
                        p.op("dve", lambda e: e.tensor_scalar_add(out=x2v, in0=x2v, scalar1=1.0), reads=[b_x2], writes=[b_x2])
                        p.op("dve", lambda e: e.reciprocal(out=x2v, in_=x2v), reads=[b_x2], writes=[b_x2])
                        p.op("dve", lambda e: e.tensor_tensor(out=hg[:, 0:255], in0=x2v, in1=xv, op=ALU.mult), reads=[b_x2, b_xh], writes=[b_hg])
                        pt2, b_pt2 = self.pb(3 + h)
                        if kv == 0:
                            p.op("pe", lambda e, pt2=pt2: e.matmul(pt2[0:64, 0:256], lhsT=w2b[:], rhs=hg[:], start=True, stop=True), reads=[b_w2b, b_hg], writes=[b_pt2])
                            p.op("act", lambda e, pt2=pt2, h=h: e.activation(out=kcmpT[:, h, :], in_=pt2[0:64, 0:256], func=AF.Copy), reads=[b_pt2], writes=[b_kcmpT])
                        else:
                            for nt in range(2):
                                p.op("pe", lambda e, pt2=pt2, nt=nt: e.matmul(pt2[:, nt * 64:(nt + 1) * 64], lhsT=hg[:, nt * 128:(nt + 1) * 128], rhs=w2b[:], start=True, stop=True), reads=[b_w2b, b_hg], writes=[b_pt2])
                            p.op("act", lambda e, pt2=pt2, h=h: e.activation(out=RCv[:, :, h, 0:64], in_=pt2[:, 0:128].rearrange("p (n d) -> p n d", n=2), func=AF.Copy), reads=[b_pt2], writes=[b_RCv])
            p.barrier()
            if "kcmpT" in self.dbg:
                p.dma("sp", lambda e: e.dma_start(out=self.S["kcmpT"], in_=kcmpT[:]), reads=[b_kcmpT], writes=[self.SB["kcmpT"]])
                p.dma("sp", lambda e: e.dma_start(out=self.S["RCv"], in_=RCv[:]), reads=[b_RCv], writes=[self.SB["RCv"]])
                p.dma("sp", lambda e: e.dma_start(out=self.S["gsig"], in_=gsig[:]), reads=b_gs, writes=[self.SB["gsig"]])
            if self.dbg.get("attn_stop") == "m1b":
                return
            with ExitStack() as p2:
                T2 = lambda name, shape, dt: self.T(p2, name, shape, dt)
                erot = Rot(self, p2, "Et", [128, 512], BF16, 4)
                qxrot = Rot(self, p2, "QX", [128, 4, 128], BF16, 2)
                oat_rot = Rot(self, p2, "oat", [128, 512], F32, 2)
                selpad, b_selpad = T2("selpad", [128, 128], F32)
                imp, b_imp = T2("imp", [128, 64], F32)
                scb, b_scb = T2("scb", [128, 64], F32)
                m8, b_m8 = T2("m8", [128, 8], F32)
                thr, b_thr = T2("thr", [128, 1], F32)
                rd, b_rd = T2("rd", [128, 3, 4], F32)
                osb_rot = Rot(self, p2, "osb", [128, 4, 65], F32, 2)
                nat, b_nat = T2("nat", [128, 512], F32)
                ssq, b_ssq = T2("ssq", [128, 2], F32)
                junk, b_junk = T2("junk", [128, 512], F32)
                onT_rot = Rot(self, p2, "onT", [128, 4, 128], BF16, 2)
                epsr, b_epsr = T2("epsr", [128, 1], F32)
                p.op("pool", lambda e: e.memset(epsr[:], RMS_EPS), writes=[b_epsr])
                p.op("pool", lambda e: e.memset(selpad[:], 0.0), writes=[b_selpad])
                self.load_bcast("sp", nat[:], b_nat, I["norm_attn"][l:l + 1, :])
                sbi = [0]
                sc2, b_sc2 = T2("sc2", [128, 64], F32)
                SK = 2

                def run_tiles(tasks, hooks):
                    ctxs = []
                    for t in range(len(tasks) + SK):
                        if t < len(tasks):
                            ctxs.append(tasks[t][0]())
                        if t >= SK:
                            tasks[t - SK][1](ctxs[t - SK])
                            if (t - SK) in hooks:
                                hooks[t - SK]()

                oats = {}

                def part1(i, h):
                    if h == 0:
                        self.emit_casts(self.attn_cast_n)
                        oats[i] = oat_rot.next()
                    oat, b_oat = oats[i]
                    qsl = slice(i * 128, (i + 1) * 128)
                    qchunk = b_qT[i // 4]
                    Oc, b_Oc = self.pb(3)
                    IM, b_IM = self.pb(4)
                    Ow, b_Ow = self.pb(6)
                    qrhs = qT[:, 4 * h:4 * h + 4, qsl]
                    QX, b_QX = qxrot.next()

                    def topk():
                        p.op("dve", lambda e: e.tensor_scalar(out=rd[:, 0, :], in0=Oc[:, 0:260].rearrange("p (g d) -> p g d", g=4)[:, :, 64], scalar1=1e-30, scalar2=None, op0=ALU.max), reads=[b_Oc], writes=[b_rd])
                        p.op("dve", lambda e: e.reciprocal(out=rd[:, 0, :], in_=rd[:, 0, :]), reads=[b_rd], writes=[b_rd])
                        p.op("dve", lambda e: e.tensor_scalar(out=imp[:], in0=IM[:, 0:64], scalar1=rd[:, 0, 0:1], scalar2=None, op0=ALU.mult), reads=[b_IM, b_rd], writes=[b_imp])
                        for g in range(1, 4):
                            p.op("dve", lambda e, g=g: e.scalar_tensor_tensor(out=imp[:], in0=IM[:, g * 64:(g + 1) * 64], scalar=rd[:, 0, g:g + 1], in1=imp[:], op0=ALU.mult, op1=ALU.add), reads=[b_IM, b_rd, b_imp], writes=[b_imp])
                        for hq in range(2):
                            cur = 2 * i + hq
                            rows = slice(hq * 64, (hq + 1) * 64)
                            p.op("pool", lambda e, rows=rows, cur=cur: e.affine_select(out=scb[rows], in_=imp[rows], pattern=[[-2, 64]], compare_op=ALU.is_ge, fill=self.regm1, base=2 * cur + 1, channel_multiplier=0), reads=[b_imp], writes=[b_scb])
                            p.op("pool", lambda e, rows=rows: e.memset(scb[rows, 0:1], 1000.0), reads=[b_scb], writes=[b_scb])
                            if cur >= 1:
                                p.op("pool", lambda e, rows=rows, cur=cur: e.memset(scb[rows, cur:cur + 1], 1001.0), reads=[b_scb], writes=[b_scb])
                            if cur >= 2:
                                p.op("pool", lambda e, rows=rows, cur=cur: e.memset(scb[rows, cur - 1:cur], 1002.0), reads=[b_scb], writes=[b_scb])
                        p.op("dve", lambda e: e.max(out=m8[:], in_=scb[:]), reads=[b_scb], writes=[b_m8])
                        p.op("dve", lambda e: e.match_replace(out=sc2[:], in_to_replace=m8[:], in_values=scb[:], imm_value=-1e9), reads=[b_scb, b_m8], writes=[b_sc2])
                        p.op("dve", lambda e: e.max(out=m8[:], in_=sc2[:]), reads=[b_sc2], writes=[b_m8])
                        p.op("dve", lambda e: e.tensor_scalar(out=thr[:], in0=m8[:, 7:8], scalar1=-0.5, scalar2=None, op0=ALU.max), reads=[b_m8], writes=[b_thr])
                        p.op("dve", lambda e: e.tensor_scalar(out=selpad[:, 64:128], in0=scb[:], scalar1=thr[:, 0:1], scalar2=1.0, op0=ALU.is_ge, op1=ALU.subtract), reads=[b_scb, b_thr], writes=[b_selpad])
                        if "seldbg" in self.dbg:
                            p.dma("sp", lambda e: e.dma_start(out=self.S["seldbg"][i, h], in_=selpad[:, 64:128]), reads=[b_selpad], writes=[self.SB["seldbg"]])
                        combine(i, h, oat, b_oat, 0, Oc, b_Oc, True)

                    tasks = []
                    nts = [0] if 8 * i + 6 < 128 else [0, 1]
                    for nt in nts:
                        tasks.append((s1(kcmpT[:, h, nt * 128:(nt + 1) * 128], qrhs, [b_kcmpT, qchunk], (128 * i - 31 - 2048 * nt, -16, [[0, 4], [1, 128]])),
                                      s2([(Oc, b_Oc, 65, RCv[:, nt, h, 0:65], [b_RCv]), (IM, b_IM, 64, ov[:, nt, :], [b_ov])], nt == 0, nt == nts[-1])))
                    hooks = {len(nts) - 1: topk}
                    kts = list(range(max(0, i - 4), i + 1))
                    for kt in kts:
                        mask = None
                        if kt == i:
                            mask = (0, -1, [[0, 4], [1, 128]])
                        elif kt == i - 4:
                            mask = (-1, 1, [[0, 4], [-1, 128]])
                        tasks.append((s1(kwT[:, h, kt * 128:(kt + 1) * 128], qrhs, [b_kwT[kt // 4], qchunk], mask),
                                      s2([(Ow, b_Ow, 65, vwA[:, kt, h, 0:65], [b_vwA[kt], b_one])], kt == kts[0], kt == kts[-1])))
                    p.op("dve", lambda e: e.tensor_copy(out=QX[0:64], in_=qrhs), reads=[qchunk], writes=[b_QX])
                    run_tiles(tasks, hooks)
                    combine(i, h, oat, b_oat, 2, Ow, b_Ow, False)
                    pm, b_pm = self.pb(7)
                    p.op("pe", lambda e: e.transpose(out=pm[:, 0:128], in_=selpad[:], identity=self.ident[:]), reads=[b_selpad, self.b_ident], writes=[b_pm])
                    p.op("act", lambda e: e.activation(out=QX[64:128], in_=pm[64:128, 0:128].unsqueeze(1).to_broadcast([64, 4, 128]), func=AF.Copy, scale=30000.0), reads=[b_pm], writes=[b_QX])
                    return QX, b_QX

                def part2(i, h, ctx):
                    QX, b_QX = ctx
                    oat, b_oat = oats[i]
                    Os, b_Os = self.pb(5)
                    tasks = []
                    for kt in range(i + 1):
                        mask = (0, -1, [[0, 4], [1, 128]]) if kt == i else None
                        tasks.append((s1(KX[:, h, kt * 128:(kt + 1) * 128], QX[:].rearrange("p g q -> p (g q)"), [b_KX[kt // 4], b_KXc, b_QX], mask),
                                      s2([(Os, b_Os, 65, vsA[:, kt, h, 0:65], [b_vsA[kt], b_one])], kt == 0, kt == i)))
                    run_tiles(tasks, {})
                    combine(i, h, oat, b_oat, 1, Os, b_Os, False)

                def finalize(i):
                    oat, b_oat = oats.pop(i)
                    qsl = slice(i * 128, (i + 1) * 128)
                    if "oattn" in self.dbg:
                        p.dma("sp", lambda e: e.dma_start(out=self.S["oattn"][qsl, :], in_=oat[:]), reads=[b_oat], writes=[self.SB["oattn"]])
                    p.op("act", lambda e: e.activation(out=junk[:], in_=oat[:], func=AF.Square, accum_out=ssq[:, 0:1]), reads=[b_oat], writes=[b_junk, b_ssq])
                    p.op("act", lambda e: e.activation(out=ssq[:, 1:2], in_=ssq[:, 0:1], func=AF.Ln, scale=1.0 / 512, bias=epsr[:]), reads=[b_ssq, b_epsr], writes=[b_ssq])
                    p.op("act", lambda e: e.activation(out=ssq[:, 1:2], in_=ssq[:, 1:2], func=AF.Exp, scale=-0.5), reads=[b_ssq], writes=[b_ssq])
                    p.op("dve", lambda e: e.scalar_tensor_tensor(out=oat[:], in0=oat[:], scalar=ssq[:, 1:2], in1=nat[:], op0=ALU.mult, op1=ALU.mult), reads=[b_oat, b_ssq, b_nat], writes=[b_oat])
                    pm, b_pm = self.pb(7)
                    for j in range(4):
                        p.op("pe", lambda e, j=j: e.transpose(out=pm[:, j * 128:(j + 1) * 128], in_=oat[:, j * 128:(j + 1) * 128], identity=self.ident[:]), reads=[b_oat, self.b_ident], writes=[b_pm])
                    onT, b_onT = onT_rot.next()
                    p.op("dve", lambda e: e.tensor_copy(out=onT[:], in_=pm[:].rearrange("p (j q) -> p j q", j=4)), reads=[b_pm], writes=[b_onT])
                    p.dma("sp", lambda e: e.dma_start(out=S["mixT"][0:512, qsl].rearrange("(j p) t -> p j t", p=128), in_=onT[:]), reads=[b_onT], writes=[self.SB["mixT"]])

                def s1(lhsT, rhs, reads, mask):
                    def f():
                        pt, b_pt = self.pb(sbi[0] % 3)
                        sbi[0] += 1
                        p.op("pe", lambda e: e.matmul(pt[:], lhsT=lhsT, rhs=rhs, start=True, stop=True), reads=reads, writes=[b_pt])
                        Et, b_Et = erot.next()
                        p.op("act", lambda e: e.activation(out=Et[:], in_=pt[:], func=AF.Exp, scale=0.125), reads=[b_pt], writes=[b_Et])
                        if mask is not None:
                            base, cm, pat = mask
                            Ev = Et[:].rearrange("p (g q) -> p g q", g=4)
                            p.op("pool", lambda e: e.affine_select(out=Ev, in_=Ev, pattern=pat, compare_op=ALU.is_ge, fill=self.reg0, base=base, channel_multiplier=cm), reads=[b_Et], writes=[b_Et])
                        return Et, b_Et
                    return f

                def s2(accs, first, last):
                    def f(ctx):
                        Et, b_Et = ctx
                        for acc, b_acc, width, rhs, reads in accs:
                            for g in range(4):
                                p.op("pe", lambda e, g=g: e.matmul(acc[:, g * width:(g + 1) * width], lhsT=Et[:, g * 128:(g + 1) * 128], rhs=rhs, start=(first and g == 0), stop=last, skip_group_check=True),
                                     reads=[b_Et] + reads, writes=[b_acc])
                    return f

                def combine(i, h, oat, b_oat, b, acc, b_acc, first):
                    av = acc[:, 0:260].rearrange("p (g d) -> p g d", g=4)
                    if b > 0:
                        p.op("dve", lambda e: e.tensor_scalar(out=rd[:, b, :], in0=av[:, :, 64], scalar1=1e-30, scalar2=None, op0=ALU.max), reads=[b_acc], writes=[b_rd])
                        p.op("dve", lambda e: e.reciprocal(out=rd[:, b, :], in_=rd[:, b, :]), reads=[b_rd], writes=[b_rd])
                    gv = gsig[:, i, h * 12:(h + 1) * 12].rearrange("p (g b) -> p g b", b=3)[:, :, b]
                    p.op("dve", lambda e: e.tensor_tensor(out=rd[:, b, :], in0=rd[:, b, :], in1=gv, op=ALU.mult), reads=[b_rd, b_gs[i]], writes=[b_rd])
                    for g in range(4):
                        osl = oat[:, h * 256 + g * 64: h * 256 + (g + 1) * 64]
                        if first:
                            p.op("dve", lambda e, g=g, osl=osl: e.tensor_scalar(out=osl, in0=av[:, g, 0:64], scalar1=rd[:, b, g:g + 1], scalar2=None, op0=ALU.mult), reads=[b_acc, b_rd], writes=[b_oat])
                        else:
                            p.op("dve", lambda e, g=g, osl=osl: e.scalar_tensor_tensor(out=osl, in0=av[:, g, 0:64], scalar=rd[:, b, g:g + 1], in1=osl, op0=ALU.mult, op1=ALU.add), reads=[b_acc, b_rd, b_oat], writes=[b_oat])

                seqs = [(i, h) for i in range(self.dbg.get("attn_nblk", NT)) for h in range(2)]
                ctx = part1(*seqs[0])
                for k, (i, h) in enumerate(seqs):
                    nxt = part1(*seqs[k + 1]) if k + 1 < len(seqs) else None
                    part2(i, h, ctx)
                    if h == 1:
                        finalize(i)
                    ctx = nxt
                if self.attn_cast_n == 7:
                    self.emit_casts()

    def mix_ssm(self, l):
        p, I, S = self.p, self.I, self.S
        MAGIC = 12582912.0
        TWO_PI_S = 6.28318
        with ExitStack() as ph:
            T = lambda name, shape, dt: self.T(ph, name, shape, dt)
            rr, b_rr = T("rr", [128, 32], F32)
            th, b_th = T("th", [128, 32], F32)
            cst, b_cst = T("cst", [128, 5], F32)
            TB1, b_TB1 = T("TB1", [128, 4, 128], F32)
            TB2, b_TB2 = T("TB2", [128, 4, 128], F32)
            TC1, b_TC1 = T("TC1", [128, 4, 128], F32)
            TC2, b_TC2 = T("TC2", [128, 4, 128], F32)
            rowm, b_rowm = T("rowm", [128, 8], F32)
            colm, b_colm = T("colm", [128, 8, 128], F32)
            dT, b_dT = T("dT", [128, 4], F32)
            inner = ExitStack()
            Ti = lambda name, shape, dt: self.T(inner, name, shape, dt)
            are, b_are = Ti("are", [128, 32], F32)
            aim, b_aim = Ti("aim", [128, 32], F32)
            dt_, b_dt = Ti("dt", [128, 32], F32)
            cs, b_cs = Ti("cs", [128, 2, 32], F32)
            t1, b_t1 = Ti("t1", [128, 32], F32)
            t2, b_t2 = Ti("t2", [128, 32], F32)
            kr, b_kr = Ti("kr", [128, 32], F32)
            ki, b_ki = Ti("ki", [128, 32], F32)
            for hh in range(2):
                p.dma("sp", lambda e, hh=hh: e.dma_start(out=are[hh * 64:(hh + 1) * 64], in_=I["ssm_a_re"][l].rearrange("g p -> p g"), allow_slow_non_contiguous=True), writes=[b_are])
                p.dma("sp", lambda e, hh=hh: e.dma_start(out=aim[hh * 64:(hh + 1) * 64], in_=I["ssm_a_im"][l].rearrange("g p -> p g"), allow_slow_non_contiguous=True), writes=[b_aim])
            self.load_bcast("sp", dt_[:], b_dt, I["ssm_log_dt"][l:l + 1, :])
            p.op("act", lambda e: e.activation(out=dt_[:], in_=dt_[:], func=AF.Exp), reads=[b_dt], writes=[b_dt])
            p.op("dve", lambda e: e.tensor_tensor(out=rr[:], in0=are[:], in1=dt_[:], op=ALU.mult), reads=[b_are, b_dt], writes=[b_rr])
            p.op("act", lambda e: e.activation(out=rr[:], in_=rr[:], func=AF.Exp), reads=[b_rr], writes=[b_rr])
            p.op("dve", lambda e: e.tensor_tensor(out=th[:], in0=aim[:], in1=dt_[:], op=ALU.mult), reads=[b_aim, b_dt], writes=[b_th])
            p.op("dve", lambda e: e.tensor_scalar(out=th[:], in0=th[:], scalar1=1.0 / (2.0 * np.pi), scalar2=None, op0=ALU.mult), reads=[b_th], writes=[b_th])

            for ci, cv in enumerate((0.25, 0.0, MAGIC, -MAGIC, 1.570795)):
                p.op("pool", lambda e, ci=ci, cv=cv: e.memset(cst[:, ci:ci + 1], cv), reads=[b_cst], writes=[b_cst])

            def sincos(out_ap, b_o, v_ap, b_v, off, tmpa, b_ta, tmpb, b_tb, n_eng="dve", mul=1.0, b_mul=None):
                oc = 0 if off == 0.25 else 1
                p.op("act", lambda e: e.activation(out=tmpa, in_=v_ap, func=AF.Identity, scale=mul, bias=cst[:, oc:oc + 1]), reads=[b_v, b_cst] + ([b_mul] if b_mul else []), writes=[b_ta])
                p.op("act", lambda e: e.activation(out=tmpb, in_=tmpa, func=AF.Identity, scale=1.0, bias=cst[:, 2:3]), reads=[b_ta, b_cst], writes=[b_tb])
                p.op("act", lambda e: e.activation(out=tmpb, in_=tmpb, func=AF.Identity, scale=1.0, bias=cst[:, 3:4]), reads=[b_tb, b_cst], writes=[b_tb])
                p.op(n_eng, lambda e: e.tensor_tensor(out=tmpb, in0=tmpb, in1=tmpa, op=ALU.subtract), reads=[b_ta, b_tb], writes=[b_tb])
                p.op("act", lambda e: e.activation(out=out_ap, in_=tmpb, func=AF.Sin, scale=-TWO_PI_S), reads=[b_tb], writes=[b_o])

            sincos(cs[:, 0, :], b_cs, th[:], b_th, 0.25, t1[:], b_t1, t2[:], b_t2)
            sincos(cs[:, 1, :], b_cs, th[:], b_th, 0.0, t1[:], b_t1, t2[:], b_t2)
            nr, b_nr = Ti("nr", [128, 32], F32)
            ni, b_ni = Ti("ni", [128, 32], F32)
            p.op("dve", lambda e: e.tensor_tensor(out=nr[:], in0=rr[:], in1=cs[:, 0, :], op=ALU.mult), reads=[b_rr, b_cs], writes=[b_nr])
            p.op("dve", lambda e: e.tensor_scalar_add(out=nr[:], in0=nr[:], scalar1=-1.0), reads=[b_nr], writes=[b_nr])
            p.op("dve", lambda e: e.tensor_tensor(out=ni[:], in0=rr[:], in1=cs[:, 1, :], op=ALU.mult), reads=[b_rr, b_cs], writes=[b_ni])
            p.op("dve", lambda e: e.tensor_tensor(out=t1[:], in0=are[:], in1=are[:], op=ALU.mult), reads=[b_are], writes=[b_t1])
            p.op("dve", lambda e: e.tensor_tensor(out=t2[:], in0=aim[:], in1=aim[:], op=ALU.mult), reads=[b_aim], writes=[b_t2])
            p.op("dve", lambda e: e.tensor_tensor(out=t1[:], in0=t1[:], in1=t2[:], op=ALU.add), reads=[b_t1, b_t2], writes=[b_t1])
            p.op("dve", lambda e: e.reciprocal(out=t1[:], in_=t1[:]), reads=[b_t1], writes=[b_t1])
            p.op("dve", lambda e: e.tensor_tensor(out=kr[:], in0=nr[:], in1=are[:], op=ALU.mult), reads=[b_nr, b_are], writes=[b_kr])
            p.op("dve", lambda e: e.tensor_tensor(out=t2[:], in0=ni[:], in1=aim[:], op=ALU.mult), reads=[b_ni, b_aim], writes=[b_t2])
            p.op("dve", lambda e: e.tensor_tensor(out=kr[:], in0=kr[:], in1=t2[:], op=ALU.add), reads=[b_kr, b_t2], writes=[b_kr])
            p.op("dve", lambda e: e.tensor_tensor(out=kr[:], in0=kr[:], in1=t1[:], op=ALU.mult), reads=[b_kr, b_t1], writes=[b_kr])
            p.op("dve", lambda e: e.tensor_tensor(out=ki[:], in0=ni[:], in1=are[:], op=ALU.mult), reads=[b_ni, b_are], writes=[b_ki])
            p.op("dve", lambda e: e.tensor_tensor(out=t2[:], in0=nr[:], in1=aim[:], op=ALU.mult), reads=[b_nr, b_aim], writes=[b_t2])
            p.op("dve", lambda e: e.tensor_tensor(out=ki[:], in0=ki[:], in1=t2[:], op=ALU.subtract), reads=[b_ki, b_t2], writes=[b_ki])
            p.op("dve", lambda e: e.tensor_tensor(out=ki[:], in0=ki[:], in1=t1[:], op=ALU.mult), reads=[b_ki, b_t1], writes=[b_ki])
            BR, b_BR = Ti("BR", [64, 32, 16], F32)
            BI, b_BI = Ti("BI", [64, 32, 16], F32)
            Bpr, b_Bpr = Ti("Bpr", [64, 32, 16], F32)
            Bpi, b_Bpi = Ti("Bpi", [64, 32, 16], F32)
            Btmp, b_Btmp = Ti("Btmp", [64, 32, 16], F32)
            p.dma("sp", lambda e: e.dma_start(out=BR[:], in_=I["ssm_b_re"][l].rearrange("g p h -> p g h")), writes=[b_BR])
            p.dma("sp", lambda e: e.dma_start(out=BI[:], in_=I["ssm_b_im"][l].rearrange("g p h -> p g h")), writes=[b_BI])
            krb = kr[0:64, :].unsqueeze(2).to_broadcast([64, 32, 16])
            kib = ki[0:64, :].unsqueeze(2).to_broadcast([64, 32, 16])
            p.op("dve", lambda e: e.tensor_tensor(out=Bpr[:], in0=BR[:], in1=krb, op=ALU.mult), reads=[b_BR, b_kr], writes=[b_Bpr])
            p.op("dve", lambda e: e.tensor_tensor(out=Btmp[:], in0=BI[:], in1=kib, op=ALU.mult), reads=[b_BI, b_ki], writes=[b_Btmp])
            p.op("dve", lambda e: e.tensor_tensor(out=Bpr[:], in0=Bpr[:], in1=Btmp[:], op=ALU.subtract), reads=[b_Bpr, b_Btmp], writes=[b_Bpr])
            p.op("dve", lambda e: e.tensor_tensor(out=Bpi[:], in0=BI[:], in1=krb, op=ALU.mult), reads=[b_BI, b_kr], writes=[b_Bpi])
            p.op("dve", lambda e: e.tensor_tensor(out=Btmp[:], in0=BR[:], in1=kib, op=ALU.mult), reads=[b_BR, b_ki], writes=[b_Btmp])
            p.op("dve", lambda e: e.tensor_tensor(out=Bpi[:], in0=Bpi[:], in1=Btmp[:], op=ALU.add), reads=[b_Bpi, b_Btmp], writes=[b_Bpi])
            for ct in range(4):
                pr, b_pr = self.pb(0 + ct % 2)
                pi_, b_pi = self.pb(2 + ct % 2)
                p.op("pe", lambda e, ct=ct, pr=pr: e.transpose(out=pr[:, 0:64], in_=Bpr[:, 8 * ct:8 * ct + 8, :].rearrange("p g h -> p (g h)"), identity=self.ident[0:64, 0:64]), reads=[b_Bpr, self.b_ident], writes=[b_pr])
                p.op("pe", lambda e, ct=ct, pi_=pi_: e.transpose(out=pi_[:, 0:64], in_=Bpi[:, 8 * ct:8 * ct + 8, :].rearrange("p g h -> p (g h)"), identity=self.ident[0:64, 0:64]), reads=[b_Bpi, self.b_ident], writes=[b_pi])
                p.op("dve", lambda e, ct=ct, pr=pr: e.tensor_copy(out=TB1[:, ct, 0:64], in_=pr[:, 0:64]), reads=[b_pr], writes=[b_TB1])
                p.op("dve", lambda e, ct=ct, pi_=pi_: e.tensor_copy(out=TB1[:, ct, 64:128], in_=pi_[:, 0:64]), reads=[b_pi], writes=[b_TB1])
                p.op("dve", lambda e, ct=ct, pi_=pi_: e.tensor_copy(out=TB2[:, ct, 0:64], in_=pi_[:, 0:64]), reads=[b_pi], writes=[b_TB2])
                p.op("dve", lambda e, ct=ct, pr=pr: e.tensor_scalar(out=TB2[:, ct, 64:128], in0=pr[:, 0:64], scalar1=-1.0, scalar2=None, op0=ALU.mult), reads=[b_pr], writes=[b_TB2])
            CR, b_CR = Ti("CR", [128, 4, 64], F32)
            CI, b_CI = Ti("CI", [128, 4, 64], F32)
            Cc1, b_Cc1 = Ti("Cc1", [128, 4, 128], F32)
            Cc2, b_Cc2 = Ti("Cc2", [128, 4, 128], F32)
            p.dma("sp", lambda e: e.dma_start(out=CR[:], in_=I["ssm_c_re"][l].rearrange("(ct g) h p -> (g h) ct p", g=8)), writes=[b_CR])
            p.dma("sp", lambda e: e.dma_start(out=CI[:], in_=I["ssm_c_im"][l].rearrange("(ct g) h p -> (g h) ct p", g=8)), writes=[b_CI])
            p.op("dve", lambda e: e.tensor_copy(out=Cc1[:, :, 0:64], in_=CR[:]), reads=[b_CR], writes=[b_Cc1])
            p.op("dve", lambda e: e.tensor_scalar(out=Cc1[:, :, 64:128], in0=CI[:], scalar1=-1.0, scalar2=None, op0=ALU.mult), reads=[b_CI], writes=[b_Cc1])
            p.op("dve", lambda e: e.tensor_scalar(out=Cc2[:, :, 0:64], in0=CI[:], scalar1=-1.0, scalar2=None, op0=ALU.mult), reads=[b_CI], writes=[b_Cc2])
            p.op("dve", lambda e: e.tensor_scalar(out=Cc2[:, :, 64:128], in0=CR[:], scalar1=-1.0, scalar2=None, op0=ALU.mult), reads=[b_CR], writes=[b_Cc2])
            for ct in range(4):
                p1_, b_p1 = self.pb(4 + ct % 2)
                p2_, b_p2 = self.pb(6 + ct % 2)
                p.op("pe", lambda e, ct=ct, p1_=p1_: e.transpose(out=p1_[:, 0:128], in_=Cc1[:, ct, :], identity=self.ident[:]), reads=[b_Cc1, self.b_ident], writes=[b_p1])
                p.op("pe", lambda e, ct=ct, p2_=p2_: e.transpose(out=p2_[:, 0:128], in_=Cc2[:, ct, :], identity=self.ident[:]), reads=[b_Cc2, self.b_ident], writes=[b_p2])
                p.op("dve", lambda e, ct=ct, p1_=p1_: e.tensor_copy(out=TC1[:, ct, :], in_=p1_[:, 0:128]), reads=[b_p1], writes=[b_TC1])
                p.op("dve", lambda e, ct=ct, p2_=p2_: e.tensor_copy(out=TC2[:, ct, :], in_=p2_[:, 0:128]), reads=[b_p2], writes=[b_TC2])
            p.op("pool", lambda e: e.memset(rowm[:], 1.0), writes=[b_rowm])
            p.op("pool", lambda e: e.affine_select(out=rowm[:], in_=rowm[:], pattern=[[-16, 8]], compare_op=ALU.is_ge, fill=self.reg0, base=0, channel_multiplier=1), reads=[b_rowm], writes=[b_rowm])
            p.op("pool", lambda e: e.affine_select(out=rowm[:], in_=rowm[:], pattern=[[16, 8]], compare_op=ALU.is_ge, fill=self.reg0, base=15, channel_multiplier=-1), reads=[b_rowm], writes=[b_rowm])
            p.op("pool", lambda e: e.memset(colm[:], 0.0), writes=[b_colm])
            for g8 in range(8):
                p.op("pool", lambda e, g8=g8: e.memset(colm[:, g8, 16 * g8:16 * g8 + 16], 1.0), reads=[b_colm], writes=[b_colm])
            p.dma("sp", lambda e: e.dma_start(out=dT[:], in_=I["ssm_d"][l].rearrange("(ct g) h -> (g h) ct", g=8), allow_slow_non_contiguous=True), writes=[b_dT])
            p.barrier()
            inner.close()
            iot, b_iot = T("iot", [128, L], F32)
            p.op("pool", lambda e: e.iota(out=iot[:], pattern=[[1, L]], base=0, channel_multiplier=0, allow_small_or_imprecise_dtypes=True), writes=[b_iot])
            ones5, b_ones5 = T("ones5", [128, 512], F32)
            p.op("pool", lambda e: e.memset(ones5[:], 1.0), writes=[b_ones5])
            ta, b_ta = T("ta", [128, L], F32)
            tb, b_tb = T("tb", [128, L], F32)
            td, b_td = T("td", [128, L], F32)
            cosrot = Rot(self, ph, "COS", [128, L], F32, 2)
            sinrot = Rot(self, ph, "SIN", [128, L], F32, 2)
            urot = Rot(self, ph, "uTt", [128, L], BF16, 2)
            yacc, b_yacc = T("yacc", [128, L], F32)
            lrot = [Rot(self, ph, nm, [128, 128], BF16, 2) for nm in ("TB1g", "TB2g", "TC1g", "TC2g")]
            RMrot = Rot(self, ph, "RM", [128, 512], F32, 2)
            m1rot = Rot(self, ph, "m1", [128, 512], F32, 3)
            m2rot = Rot(self, ph, "m2", [128, 512], F32, 3)
            zrot = Rot(self, ph, "zs", [128, 512], F32, 5)
            xcrot = Rot(self, ph, "xc", [128, 512], BF16, 2)
            xsrot = Rot(self, ph, "xs_", [128, 512], BF16, 2)
            bk = [0]

            b_tbh = [Buf("tbh0"), Buf("tbh1")]
            b_tdh = [Buf("tdh0"), Buf("tdh1")]

            class TabGen:
                def __init__(tg_, g):
                    tg_.COS, _ = cosrot.next()
                    tg_.SIN, _ = sinrot.next()
                    tg_.b_COS = [Buf("COSh0"), Buf("COSh1")]
                    tg_.b_SIN = [Buf("SINh0"), Buf("SINh1")]
                    p.op("act", lambda e: e.activation(out=ta[:], in_=iot[:], func=AF.Identity, scale=th[:, g:g + 1], bias=cst[:, 1:2]), reads=[b_iot, b_cst, b_th], writes=[b_ta])
                    p.op("act", lambda e: e.activation(out=tb[:], in_=ta[:], func=AF.Identity, scale=1.0, bias=cst[:, 2:3]), reads=[b_ta, b_cst], writes=b_tbh)
                    p.op("act", lambda e: e.activation(out=tb[:], in_=tb[:], func=AF.Identity, scale=1.0, bias=cst[:, 3:4]), reads=b_tbh + [b_cst], writes=b_tbh)

                def step(tg_, k):
                    hf = k // 4
                    csl = slice(k * 512, (k + 1) * 512)
                    p.op("pool", lambda e: e.tensor_tensor(out=tb[:, csl], in0=tb[:, csl], in1=ta[:, csl], op=ALU.subtract), reads=[b_ta, b_tbh[hf]], writes=[b_tbh[hf]])
                    if k % 4 == 3:
                        hsl = slice(hf * 2048, (hf + 1) * 2048)
                        COS, SIN = tg_.COS, tg_.SIN
                        p.op("act", lambda e: e.activation(out=SIN[:, hsl], in_=tb[:, hsl], func=AF.Sin, scale=-TWO_PI_S), reads=[b_tbh[hf]], writes=[tg_.b_SIN[hf], slotb[id(SIN)][hf]])
                        p.op("act", lambda e: e.activation(out=td[:, hsl], in_=tb[:, hsl], func=AF.Abs), reads=[b_tbh[hf]], writes=[b_tdh[hf]])
                        p.op("act", lambda e: e.activation(out=COS[:, hsl], in_=td[:, hsl], func=AF.Sin, scale=-TWO_PI_S, bias=cst[:, 4:5]), reads=[b_tdh[hf], b_cst], writes=[tg_.b_COS[hf], slotb[id(COS)][hf]])

            slotb = {}
            for r_ in (cosrot, sinrot):
                for t_, _b in r_.items:
                    slotb[id(t_)] = [Buf("slot0"), Buf("slot1")]

            tabs = {0: TabGen(0)}
            for k in range(8):
                tabs[0].step(k)
            for ct in range(4):
                uTt, b_uTt = urot.next()
                p.dma("sp", lambda e, uTt=uTt, ct=ct: e.dma_start(out=uTt[:], in_=S["uT"][ct * 128:(ct + 1) * 128, :]), reads=[self.SB["uT"]], writes=[b_uTt])
                for g8 in range(8):
                    g = ct * 8 + g8
                    tgc = tabs.pop(g)
                    COS, SIN = tgc.COS, tgc.SIN
                    self.emit_casts(2)
                    (TB1g, b_1), (TB2g, b_2), (TC1g, b_3), (TC2g, b_4) = [r_.next() for r_ in lrot]
                    p.op("dve", lambda e: e.tensor_scalar(out=TB1g[:], in0=TB1[:, ct, :], scalar1=rowm[:, g8:g8 + 1], scalar2=None, op0=ALU.mult), reads=[b_TB1, b_rowm], writes=[b_1])
                    p.op("dve", lambda e: e.tensor_scalar(out=TB2g[:], in0=TB2[:, ct, :], scalar1=rowm[:, g8:g8 + 1], scalar2=None, op0=ALU.mult), reads=[b_TB2, b_rowm], writes=[b_2])
                    p.op("dve", lambda e: e.tensor_tensor(out=TC1g[:], in0=TC1[:, ct, :], in1=colm[:, g8, :], op=ALU.mult), reads=[b_TC1, b_colm], writes=[b_3])
                    p.op("dve", lambda e: e.tensor_tensor(out=TC2g[:], in0=TC2[:, ct, :], in1=colm[:, g8, :], op=ALU.mult), reads=[b_TC2, b_colm], writes=[b_4])
                    RM, b_RM = RMrot.next()
                    p.op("dve", lambda e: e.tensor_scalar(out=RM[:], in0=ones5[:], scalar1=rr[:, g:g + 1], scalar2=None, op0=ALU.mult), reads=[b_ones5, b_rr], writes=[b_RM])
                    if g + 1 < 32:
                        tabs[g + 1] = TabGen(g + 1)
                    zprev = [None]

                    def stage_a1(tc):
                        csl = slice(tc * 512, (tc + 1) * 512)
                        P1, b_P1 = self.pb(bk[0] % 2)
                        P2, b_P2 = self.pb(2 + bk[0] % 2)
                        bk[0] += 1
                        p.op("pe", lambda e: e.matmul(P1[:], lhsT=TB1g[:], rhs=uTt[:, csl], start=True, stop=True), reads=[b_1, b_uTt], writes=[b_P1])
                        p.op("pe", lambda e: e.matmul(P2[:], lhsT=TB2g[:], rhs=uTt[:, csl], start=True, stop=True), reads=[b_2, b_uTt], writes=[b_P2])
                        m1, b_m1 = m1rot.next()
                        m2, b_m2 = m2rot.next()
                        p.op("dve", lambda e: e.tensor_tensor(out=m1[:], in0=COS[:, csl], in1=P1[:], op=ALU.mult), reads=[tgc.b_COS[tc // 4], slotb[id(COS)][tc // 4], b_P1], writes=[b_m1])
                        p.op("dve", lambda e: e.tensor_tensor(out=m2[:], in0=SIN[:, csl], in1=P2[:], op=ALU.mult), reads=[tgc.b_SIN[tc // 4], slotb[id(SIN)][tc // 4], b_P2], writes=[b_m2])
                        p.op("pool", lambda e: e.tensor_tensor(out=m1[:], in0=m1[:], in1=m2[:], op=ALU.add), reads=[b_m1, b_m2], writes=[b_m1])
                        return m1, b_m1

                    def stage_a2(tc, mm):
                        m1, b_m1 = mm
                        z, b_z = zrot.next()
                        init = 0.0 if zprev[0] is None else zprev[0][0][:, 511:512]
                        rds = [b_RM, b_m1] + ([] if zprev[0] is None else [zprev[0][1]])
                        p.op("dve", lambda e: e.tensor_tensor_scan(out=z[:], data0=RM[:], data1=m1[:], initial=init, op0=ALU.mult, op1=ALU.add), reads=rds, writes=[b_z])
                        zprev[0] = (z, b_z)
                        return z, b_z

                    def stage_b1(tc, zz):
                        z, b_z = zz
                        csl = slice(tc * 512, (tc + 1) * 512)
                        Y, b_Y = self.pb(4 + tc % 4)
                        xc, b_xc = xcrot.next()
                        xs_, b_xs = xsrot.next()
                        p.op("dve", lambda e: e.tensor_tensor(out=xc[:], in0=COS[:, csl], in1=z[:], op=ALU.mult), reads=[tgc.b_COS[tc // 4], slotb[id(COS)][tc // 4], b_z], writes=[b_xc])
                        p.op("pool", lambda e: e.tensor_tensor(out=xs_[:], in0=SIN[:, csl], in1=z[:], op=ALU.mult), reads=[tgc.b_SIN[tc // 4], slotb[id(SIN)][tc // 4], b_z], writes=[b_xs])
                        p.op("pe", lambda e: e.matmul(Y[:], lhsT=TC1g[:], rhs=xc[:], start=True, stop=False), reads=[b_3, b_xc], writes=[b_Y])
                        p.op("pe", lambda e: e.matmul(Y[:], lhsT=TC2g[:], rhs=xs_[:], start=False, stop=True), reads=[b_4, b_xs], writes=[b_Y])
                        return Y, b_Y

                    def stage_b2(tc, yy):
                        Y, b_Y = yy
                        csl = slice(tc * 512, (tc + 1) * 512)
                        if g8 == 0:
                            p.op("act", lambda e: e.activation(out=yacc[:, csl], in_=Y[:], func=AF.Copy), reads=[b_Y], writes=[b_yacc])
                        else:
                            p.op("dve", lambda e: e.tensor_tensor(out=yacc[:, csl], in0=yacc[:, csl], in1=Y[:], op=ALU.add), reads=[b_Y, b_yacc], writes=[b_yacc])

                    ms, zs, ys = {}, {}, {}
                    for tc in range(12):
                        if tc < 8:
                            ms[tc] = stage_a1(tc)
                            if g + 1 < 32:
                                tabs[g + 1].step(tc)
                        if 1 <= tc < 9:
                            zs[tc - 1] = stage_a2(tc - 1, ms.pop(tc - 1))
                        if 2 <= tc < 10:
                            ys[tc - 2] = stage_b1(tc - 2, zs.pop(tc - 2))
                        if 3 <= tc < 11:
                            stage_b2(tc - 3, ys.pop(tc - 3))
                p.op("dve", lambda e: e.scalar_tensor_tensor(out=yacc[:], in0=uTt[:], scalar=dT[:, ct:ct + 1], in1=yacc[:], op0=ALU.mult, op1=ALU.add), reads=[b_uTt, b_dT, b_yacc], writes=[b_yacc])
                p.dma("sp", lambda e, ct=ct: e.dma_start(out=S["ysT"][ct * 128:(ct + 1) * 128, :], in_=yacc[:]), reads=[b_yacc], writes=[self.SB["ysT"]])

    def mix_glu(self, l):
        p, I, S = self.p, self.I, self.S
        with ExitStack() as ph:
            T = lambda name, shape, dt: self.T(ph, name, shape, dt)
            wgl, b_wgl = T("wgl", [128, 4, 512], BF16)
            wn = "w_glu_b%d" % l
            p.dma("sp", lambda e: e.dma_start(out=wgl[:], in_=S[wn].rearrange("(ct p) n -> p ct n", p=128)), reads=[self.SB[wn]], writes=[b_wgl])
            nsT, b_nsT = T("nsT", [128, 4], F32)
            p.dma("sp", lambda e: e.dma_start(out=nsT[:], in_=I["norm_ssm"][l].rearrange("(ct p) -> p ct", p=128), allow_slow_non_contiguous=True), writes=[b_nsT])
            epsr, b_epsr = T("epsr2", [128, 1], F32)
            p.op("pool", lambda e: e.memset(epsr[:], RMS_EPS), writes=[b_epsr])
            yrot = Rot(self, ph, "y4", [128, 4, 512], F32, 2)
            x2rot = Rot(self, ph, "gx2", [128, 4, 512], F32, 2)
            ygbrot = Rot(self, ph, "ygb", [128, 4, 512], BF16, 2)
            sgrot = Rot(self, ph, "sg", [128, 512], F32, 2)
            sqrot = Rot(self, ph, "sq", [128, 512], F32, 2)
            rsrot = Rot(self, ph, "rs5", [128, 512], F32, 2)
            onrot = Rot(self, ph, "onb", [128, 4, 512], BF16, 2)
            def glu_a(tc):
                self.emit_casts(4)
                csl = slice(tc * 512, (tc + 1) * 512)
                y4, b_y4 = yrot.next()
                x2, b_x2 = x2rot.next()
                ygb, b_ygb = ygbrot.next()
                p.dma("sp", lambda e: e.dma_start(out=y4[:], in_=S["ysT"][:, csl].rearrange("(ct p) t -> p ct t", p=128)), reads=[self.SB["ysT"]], writes=[b_y4])
                p.op("pool", lambda e: e.tensor_tensor(out=x2[:], in0=y4[:], in1=y4[:], op=ALU.mult), reads=[b_y4], writes=[b_x2])
                p.op("dve", lambda e: e.tensor_scalar(out=x2[:], in0=x2[:], scalar1=0.044715, scalar2=1.0, op0=ALU.mult, op1=ALU.add), reads=[b_x2], writes=[b_x2])
                p.op("pool", lambda e: e.tensor_tensor(out=x2[:], in0=x2[:], in1=y4[:], op=ALU.mult), reads=[b_x2, b_y4], writes=[b_x2])
                p.op("act", lambda e: e.activation(out=x2[:], in_=x2[:], func=AF.Exp, scale=-1.5957691216), reads=[b_x2], writes=[b_x2])
                p.op("dve", lambda e: e.tensor_scalar_add(out=x2[:], in0=x2[:], scalar1=1.0), reads=[b_x2], writes=[b_x2])
                p.op("dve", lambda e: e.reciprocal(out=x2[:], in_=x2[:]), reads=[b_x2], writes=[b_x2])
                p.op("dve", lambda e: e.tensor_tensor(out=y4[:], in0=y4[:], in1=x2[:], op=ALU.mult), reads=[b_x2, b_y4], writes=[b_y4])
                p.op("act", lambda e: e.activation(out=ygb[:], in_=y4[:], func=AF.Copy), reads=[b_y4], writes=[b_ygb])
                return y4, b_y4, ygb, b_ygb

            def glu_b(tc, c):
                y4, b_y4, ygb, b_ygb = c
                csl = slice(tc * 512, (tc + 1) * 512)
                pss, b_pss = self.pb(6 + tc % 2)
                for c2 in range(4):
                    pz, b_pz = self.pb(c2)
                    for ct in range(4):
                        p.op("pe", lambda e, ct=ct: e.matmul(pz[:], lhsT=wgl[:, ct, c2 * 128:(c2 + 1) * 128], rhs=ygb[:, ct, :], start=(ct == 0), stop=(ct == 3)), reads=[b_wgl, b_ygb], writes=[b_pz])
                    sg, b_sg = sgrot.next()
                    p.op("act", lambda e: e.activation(out=sg[:], in_=pz[:], func=AF.Exp, scale=-1.0), reads=[b_pz], writes=[b_sg])
                    p.op("dve", lambda e: e.tensor_scalar_add(out=sg[:], in0=sg[:], scalar1=1.0), reads=[b_sg], writes=[b_sg])
                    p.op("dve", lambda e: e.reciprocal(out=sg[:], in_=sg[:]), reads=[b_sg], writes=[b_sg])
                    p.op("dve", lambda e: e.tensor_tensor(out=y4[:, c2, :], in0=y4[:, c2, :], in1=sg[:], op=ALU.mult), reads=[b_sg, b_y4], writes=[b_y4])
                    sq, b_sq = sqrot.next()
                    p.op("pool", lambda e: e.tensor_tensor(out=sq[:], in0=y4[:, c2, :], in1=y4[:, c2, :], op=ALU.mult), reads=[b_y4], writes=[b_sq])
                    p.op("pe", lambda e: e.matmul(pss[:], lhsT=self.ones[:], rhs=sq[:], start=(c2 == 0), stop=(c2 == 3)), reads=[self.b_ones, b_sq], writes=[b_pss])
                rs, b_rs = rsrot.next()
                p.op("act", lambda e: e.activation(out=rs[:], in_=pss[:], func=AF.Ln, scale=1.0 / 512, bias=epsr[:]), reads=[b_pss, b_epsr], writes=[b_rs])
                p.op("act", lambda e: e.activation(out=rs[:], in_=rs[:], func=AF.Exp, scale=-0.5), reads=[b_rs], writes=[b_rs])
                onb, b_onb = onrot.next()
                for c2 in range(4):
                    p.op("dve", lambda e: e.scalar_tensor_tensor(out=onb[:, c2, :], in0=y4[:, c2, :], scalar=nsT[:, c2:c2 + 1], in1=rs[:], op0=ALU.mult, op1=ALU.mult), reads=[b_y4, b_nsT, b_rs], writes=[b_onb])
                p.dma("sp", lambda e: e.dma_start(out=S["mixT"][512:1024, csl].rearrange("(ct p) t -> p ct t", p=128), in_=onb[:]), reads=[b_onb], writes=[self.SB["mixT"]])

            ga = {0: glu_a(0)}
            for tc in range(8):
                if tc + 1 < 8:
                    ga[tc + 1] = glu_a(tc + 1)
                glu_b(tc, ga.pop(tc))

    def mix_out(self, l, src, b_src):
        p, I, S = self.p, self.I, self.S
        with ExitStack() as ph:
            T = lambda name, shape, dt: self.T(ph, name, shape, dt)
            wo, b_wo = T("wo", [128, 8, 1024], BF16)
            wn = "w_out_b%d" % l
            p.dma("sp", lambda e: e.dma_start(out=wo[:], in_=S[wn].rearrange("(kt p) n -> p kt n", p=128)), reads=[self.SB[wn]], writes=[b_wo])
            gB, b_gB = T("gB1", [128, 1024], F32)
            lng, b_lng = T("lng1", [128, 1024], F32)
            lnb, b_lnb = T("lnb1", [128, 1024], F32)
            self.load_bcast("sp", gB[:], b_gB, S["modrow"][l:l + 1, 2 * 1024:3 * 1024], reads=[self.SB["modrow"]])
            self.load_bcast("sp", lng[:], b_lng, I["ln_g"][l, 0:1, :])
            self.load_bcast("sp", lnb[:], b_lnb, I["ln_b"][l, 0:1, :])
            tmp = self.ln_tmps(ph)
            mrot = Rot(self, ph, "mixc", [128, 8, 512], BF16, 2)
            xrot = Rot(self, ph, "xin", [128, 1024], F32, 4)
            zrot = Rot(self, ph, "zt", [128, 1024], F32, 4)
            bk = [0]
            mix = {}

            def o_pre(TT):
                self.emit_casts(1)
                tc, tt = TT // 4, TT % 4
                t0 = TT * 128
                if tt == 0:
                    csl = slice(tc * 512, (tc + 1) * 512)
                    mix[tc] = mrot.next()
                    p.dma("sp", lambda e: e.dma_start(out=mix[tc][0][:], in_=S["mixT"][:, csl].rearrange("(kt p) t -> p kt t", p=128)), reads=[self.SB["mixT"]], writes=[mix[tc][1]])
                mixc, b_mixc = mix[tc]
                xt, b_xt = xrot.next()
                zt, b_zt = zrot.next()
                p.dma("sp", lambda e: e.dma_start(out=xt[:], in_=src[t0:t0 + 128, :]), reads=[b_src[TT]], writes=[b_xt])
                for hf in range(2):
                    py, b_py = self.pb(bk[0] % 4)
                    bk[0] += 1
                    for kt in range(8):
                        p.op("pe", lambda e, kt=kt: e.matmul(py[:], lhsT=mixc[:, kt, tt * 128:(tt + 1) * 128], rhs=wo[:, kt, hf * 512:(hf + 1) * 512], start=(kt == 0), stop=(kt == 7)), reads=[b_mixc, b_wo], writes=[b_py])
                    p.op("dve", lambda e: e.tensor_tensor(out=zt[:, hf * 512:(hf + 1) * 512], in0=py[:], in1=gB[:, hf * 512:(hf + 1) * 512], op=ALU.mult), reads=[b_py, b_gB], writes=[b_zt])
                if "mixdbg" in self.dbg:
                    p.dma("sp", lambda e: e.dma_start(out=self.S["mixdbg"][t0:t0 + 128, :], in_=zt[:]), reads=[b_zt], writes=[self.SB["mixdbg"]])
                p.op("dve", lambda e: e.scalar_tensor_tensor(out=zt[:], in0=xt[:], scalar=ALPHA, in1=zt[:], op0=ALU.mult, op1=ALU.add), reads=[b_xt, b_zt], writes=[b_zt])
                return xt, b_xt, zt, b_zt, self.ln_pre(zt, b_zt, tmp)

            def o_post(TT, c):
                t0 = TT * 128
                xt, b_xt, zt, b_zt, slot = c
                self.ln_post(zt, b_zt, slot, lng, b_lng, lnb, b_lnb, xt, b_xt)
                p.dma("sp", lambda e: e.dma_start(out=S["xs"][t0:t0 + 128, :], in_=xt[:]), reads=[b_xt], writes=[self.b_xs[TT]])

            cs_ = {}
            for TT in range(NT + 2):
                if TT < NT:
                    cs_[TT] = o_pre(TT)
                if TT >= 2:
                    o_post(TT - 2, cs_.pop(TT - 2))


_NC_CACHE = {}


def kernel(**inputs):
    if "nc" not in _NC_CACHE:
        _NC_CACHE["nc"] = MK().build()
    nc = _NC_CACHE["nc"]
    in_maps = []
    for b in range(8):
        m = {}
        for k in WSPEC:
            a = np.asarray(inputs[k], dtype=np.float32)
            if k in ("x", "c"):
                a = a[b]
            m[k] = np.ascontiguousarray(a)
        in_maps.append(m)
    res = run_bass_kernel_spmd(nc, in_maps, core_ids=list(range(8)))
    return np.stack([np.asarray(r["out"]) for r in res.results], axis=0).astype(np.float32)
```
